# Optimizing a Trainium2 kernel written in Bass

```python
import jax, jax.numpy as jnp
from jax import lax
import numpy as np

D_MODEL = 2048
BATCH = 1
SEQ = 8192
DEPTH = 4

N_MIXERS = 2
N_HEADS = 16
HEAD_DIM = D_MODEL // N_HEADS
ROT_DIM = HEAD_DIM // 4
ROPE_THETA = 500000.0
EPS = 1e-6
A_KV_HEADS = 4
IDX_HEADS = 16
IDX_DIM = 64
IDX_ROT_DIM = IDX_DIM // 4
IDX_TOPK_MAX = 256
A_QBLOCK = 128
A_IN = N_HEADS * HEAD_DIM + 2 * A_KV_HEADS * HEAD_DIM + IDX_HEADS * IDX_DIM + IDX_DIM + IDX_HEADS
MOBA_BLOCK = 256
MOBA_TOPK = 3
B_QBLOCK = 32
B_IN = 3 * N_HEADS * HEAD_DIM
D_FF = -(-8 * D_MODEL // (3 * 256)) * 256
N_A = (DEPTH + 1) // 2
N_B = DEPTH // 2

kernel_name = "hybrid_dsa_moba_sandwich_adaln"


def rms_norm(x, g):
    xf = x.astype(jnp.float32)
    y = xf * lax.rsqrt(jnp.mean(xf * xf, axis=-1, keepdims=True) + EPS)
    return (y * g.astype(jnp.float32)).astype(x.dtype)


def partial_rope(x, positions, rot_dim):
    half = rot_dim // 2
    inv_freq = ROPE_THETA ** (-jnp.arange(half, dtype=jnp.float32) / half)
    ang = positions.astype(jnp.float32)[:, :, None] * inv_freq
    cos = jnp.cos(ang)[:, :, None, :]
    sin = jnp.sin(ang)[:, :, None, :]
    xf = x.astype(jnp.float32)
    x1 = xf[..., :half]
    x2 = xf[..., half:rot_dim]
    out = jnp.concatenate([x1 * cos - x2 * sin, x2 * cos + x1 * sin, xf[..., rot_dim:]], axis=-1)
    return out.astype(x.dtype)


def dsa_mixer(h, positions, w_in, w_o):
    B, S, _ = h.shape
    sizes = [N_HEADS * HEAD_DIM, A_KV_HEADS * HEAD_DIM, A_KV_HEADS * HEAD_DIM,
             IDX_HEADS * IDX_DIM, IDX_DIM]
    cuts = [int(v) for v in np.cumsum(sizes)]
    q, k, v, qi, ki, wi = jnp.split(h @ w_in, cuts, axis=-1)
    q = partial_rope(q.reshape(B, S, N_HEADS, HEAD_DIM), positions, ROT_DIM)
    k = partial_rope(k.reshape(B, S, A_KV_HEADS, HEAD_DIM), positions, ROT_DIM)
    v = v.reshape(B, S, A_KV_HEADS, HEAD_DIM)
    qi = partial_rope(qi.reshape(B, S, IDX_HEADS, IDX_DIM), positions, IDX_ROT_DIM)
    ki = partial_rope(ki.reshape(B, S, 1, IDX_DIM), positions, IDX_ROT_DIM)[:, :, 0]
    wi = wi.astype(jnp.float32) * (IDX_HEADS ** -0.5 * IDX_DIM ** -0.5)
    n_sel = min(IDX_TOPK_MAX, S // 4)
    rep = N_HEADS // A_KV_HEADS
    scale = HEAD_DIM ** -0.5
    key_pos = jnp.arange(S)

    def block(i):
        start = i * A_QBLOCK
        qpos = start + jnp.arange(A_QBLOCK)
        qb = lax.dynamic_slice_in_dim(q, start, A_QBLOCK, axis=1)
        qib = lax.dynamic_slice_in_dim(qi, start, A_QBLOCK, axis=1)
        wib = lax.dynamic_slice_in_dim(wi, start, A_QBLOCK, axis=1)
        dots = jnp.einsum('bqhd,bsd->bqhs', qib, ki, preferred_element_type=jnp.float32)
        isc = jnp.einsum('bqhs,bqh->bqs', jax.nn.relu(dots), wib)
        causal = key_pos[None, :] <= qpos[:, None]
        isc = jnp.where(causal[None], isc, -jnp.inf)
        _, sel = lax.top_k(isc, n_sel)
        valid = sel <= qpos[None, :, None]
        kg = jax.vmap(lambda kb, ib: kb[ib])(k, sel)
        vg = jax.vmap(lambda vb, ib: vb[ib])(v, sel)
        qg = qb.reshape(B, A_QBLOCK, A_KV_HEADS, rep, HEAD_DIM)
        logits = jnp.einsum('bqgrd,bqkgd->bqgrk', qg, kg, preferred_element_type=jnp.float32) * scale
        logits = jnp.where(valid[:, :, None, None, :], logits, -jnp.inf)
        p = jax.nn.softmax(logits, axis=-1)
        o = jnp.einsum('bqgrk,bqkgd->bqgrd', p.astype(vg.dtype), vg)
        return o.reshape(B, A_QBLOCK, N_HEADS * HEAD_DIM)

    out = lax.map(block, jnp.arange(S // A_QBLOCK))
    out = jnp.transpose(out, (1, 0, 2, 3)).reshape(B, S, N_HEADS * HEAD_DIM)
    return out @ w_o


def moba_mixer(h, positions, w_in, w_o):
    B, S, _ = h.shape
    q, k, v = jnp.split(h @ w_in, 3, axis=-1)
    q = partial_rope(q.reshape(B, S, N_HEADS, HEAD_DIM), positions, ROT_DIM)
    k = partial_rope(k.reshape(B, S, N_HEADS, HEAD_DIM), positions, ROT_DIM)
    v = v.reshape(B, S, N_HEADS, HEAD_DIM)
    nb = -(-S // MOBA_BLOCK)
    pad = nb * MOBA_BLOCK - S
    kp = jnp.pad(k, ((0, 0), (0, pad), (0, 0), (0, 0)))
    vp = jnp.pad(v, ((0, 0), (0, pad), (0, 0), (0, 0)))
    kblk = kp.reshape(B, nb, MOBA_BLOCK, N_HEADS, HEAD_DIM)
    vblk = vp.reshape(B, nb, MOBA_BLOCK, N_HEADS, HEAD_DIM)
    kmean = jnp.mean(kblk.astype(jnp.float32), axis=2)
    k_bh = jnp.transpose(kblk, (0, 3, 1, 2, 4))
    v_bh = jnp.transpose(vblk, (0, 3, 1, 2, 4))
    n_sel = min(MOBA_TOPK, nb)
    scale = HEAD_DIM ** -0.5
    blk_ids = jnp.arange(nb)
    bi = jnp.arange(B)[:, None, None, None]
    hi = jnp.arange(N_HEADS)[None, None, :, None]

    def block(i):
        start = i * B_QBLOCK
        qpos = start + jnp.arange(B_QBLOCK)
        j = start // MOBA_BLOCK
        qb = lax.dynamic_slice_in_dim(q, start, B_QBLOCK, axis=1)
        gate = jnp.einsum('bqhd,bnhd->bqhn', qb.astype(jnp.float32), kmean)
        gate = jnp.where((blk_ids < j)[None, None, None, :], gate, -jnp.inf)
        _, sel = lax.top_k(gate, n_sel)
        sel_valid = sel < j
        kg = k_bh[bi, hi, sel]
        vg = v_bh[bi, hi, sel]
        past = jnp.einsum('bqhd,bqhnkd->bqhnk', qb, kg, preferred_element_type=jnp.float32) * scale
        past = jnp.where(sel_valid[..., None], past, -jnp.inf).reshape(B, B_QBLOCK, N_HEADS, n_sel * MOBA_BLOCK)
        own_k = lax.dynamic_slice_in_dim(kp, j * MOBA_BLOCK, MOBA_BLOCK, axis=1)
        own_v = lax.dynamic_slice_in_dim(vp, j * MOBA_BLOCK, MOBA_BLOCK, axis=1)
        own = jnp.einsum('bqhd,bkhd->bqhk', qb, own_k, preferred_element_type=jnp.float32) * scale
        own_pos = j * MOBA_BLOCK + jnp.arange(MOBA_BLOCK)
        own = jnp.where((own_pos[None, :] <= qpos[:, None])[None, :, None, :], own, -jnp.inf)
        p = jax.nn.softmax(jnp.concatenate([past, own], axis=-1), axis=-1)
        p_past = p[..., :n_sel * MOBA_BLOCK].reshape(B, B_QBLOCK, N_HEADS, n_sel, MOBA_BLOCK).astype(vg.dtype)
        p_own = p[..., n_sel * MOBA_BLOCK:].astype(own_v.dtype)
        o = (jnp.einsum('bqhnk,bqhnkd->bqhd', p_past, vg)
             + jnp.einsum('bqhk,bkhd->bqhd', p_own, own_v))
        return o.reshape(B, B_QBLOCK, N_HEADS * HEAD_DIM)

    out = lax.map(block, jnp.arange(S // B_QBLOCK))
    out = jnp.transpose(out, (1, 0, 2, 3)).reshape(B, S, N_HEADS * HEAD_DIM)
    return out @ w_o


def swiglu(h, w_in, w_out):
    u, g = jnp.split(h @ w_in, 2, axis=-1)
    return (jax.nn.silu(g) * u) @ w_out


def setup_inputs(seed: int = 0) -> dict:
    key = jax.random.key(seed)
    ks = jax.random.split(key, 13)
    f32 = jnp.float32
    hd = N_HEADS * HEAD_DIM
    x = jax.random.normal(ks[0], (BATCH, SEQ, D_MODEL), f32)
    c = jax.random.normal(ks[1], (BATCH, D_MODEL), f32)
    start = jax.random.randint(ks[2], (BATCH, 1), 0, 4096, dtype=jnp.int32)
    positions = start + jnp.arange(SEQ, dtype=jnp.int32)[None, :]
    a_w_in = jax.random.normal(ks[3], (N_A, D_MODEL, A_IN), f32) * D_MODEL ** -0.5
    a_w_o = jax.random.normal(ks[4], (N_A, hd, D_MODEL), f32) * hd ** -0.5
    b_w_in = jax.random.normal(ks[5], (N_B, D_MODEL, B_IN), f32) * D_MODEL ** -0.5
    b_w_o = jax.random.normal(ks[6], (N_B, hd, D_MODEL), f32) * hd ** -0.5
    ada_w = jax.random.normal(ks[7], (DEPTH, D_MODEL, 6 * D_MODEL), f32) * (0.5 * D_MODEL ** -0.5)
    ada_b = jax.random.normal(ks[8], (DEPTH, 6 * D_MODEL), f32) * 0.01
    norm_g = 1.0 + 0.02 * jax.random.normal(ks[9], (DEPTH, 4, D_MODEL), f32)
    ffn_w_in = jax.random.normal(ks[10], (DEPTH, D_MODEL, 2 * D_FF), f32) * D_MODEL ** -0.5
    ffn_w_out = jax.random.normal(ks[11], (DEPTH, D_FF, D_MODEL), f32) * D_FF ** -0.5
    return {"x": x, "c": c, "positions": positions,
            "a_w_in": a_w_in, "a_w_o": a_w_o, "b_w_in": b_w_in, "b_w_o": b_w_o,
            "ada_w": ada_w, "ada_b": ada_b, "norm_g": norm_g,
            "ffn_w_in": ffn_w_in, "ffn_w_out": ffn_w_out}


def reference(x, c, positions, a_w_in, a_w_o, b_w_in, b_w_o, ada_w, ada_b, norm_g, ffn_w_in, ffn_w_out):
    c_act = jax.nn.silu(c)
    for i in range(DEPTH):
        mod = (c_act @ ada_w[i] + ada_b[i])[:, None, :]
        sh1, sc1, g1, sh2, sc2, g2 = jnp.split(mod, 6, axis=-1)
        h = rms_norm(x, norm_g[i, 0]) * (1 + sc1) + sh1
        if i % N_MIXERS == 0:
            y = dsa_mixer(h, positions, a_w_in[i // 2], a_w_o[i // 2])
        else:
            y = moba_mixer(h, positions, b_w_in[i // 2], b_w_o[i // 2])
        x = x + g1 * rms_norm(y, norm_g[i, 1])
        h = rms_norm(x, norm_g[i, 2]) * (1 + sc2) + sh2
        y = swiglu(h, ffn_w_in[i], ffn_w_out[i])
        x = x + g2 * rms_norm(y, norm_g[i, 3])
    return x
```

```python
import numpy as np
import ml_dtypes
from contextlib import ExitStack
import concourse.bass as bass
import concourse.mybir as mybir
from concourse.bass_utils import run_bass_kernel_spmd

F32 = mybir.dt.float32
BF16 = mybir.dt.bfloat16
I32 = mybir.dt.int32
U8 = mybir.dt.uint8
AF = mybir.ActivationFunctionType
ALU = mybir.AluOpType
AX = mybir.AxisListType

NCORES = 8
D = 2048
KD = 16
S = 8192
TL = 1024
DFF = 5632
NF = 44
EPS = 1e-6
CW = 256
NDC = CW // 128


class SemC:
    def __init__(self, h):
        self.h = h
        self.count = 0


class Tok:
    __slots__ = ("sem", "val")

    def __init__(self, sem, val):
        self.sem = sem
        self.val = val


class Ctx:
    def __init__(self, nc, es):
        self.nc = nc
        self.es = es
        self.engs = {"pe": nc.tensor, "act": nc.scalar, "dve": nc.vector, "pool": nc.gpsimd, "sp": nc.sync}
        self.esem = {n: self.sem("s_" + n) for n in ("pe", "act", "dve", "pool")}
        self.waited = {}
        self.nsem = 0

    def sem(self, name):
        return SemC(self.es.enter_context(self.nc.semaphore(name)))

    def sb(self, name, shape, dt):
        return self.es.enter_context(self.nc.sbuf_tensor(name, shape, dt))

    def _flat(self, toks, acc):
        for t in toks:
            if t is None:
                continue
            if isinstance(t, (list, tuple)):
                self._flat(t, acc)
            else:
                k = id(t.sem)
                if k not in acc or acc[k].val < t.val:
                    acc[k] = t
        return acc

    def wait(self, eng, toks):
        for t in self._flat(toks, {}).values():
            key = (eng, id(t.sem))
            if self.waited.get(key, 0) >= t.val:
                continue
            self.engs[eng].wait_ge(t.sem.h, t.val)
            self.waited[key] = t.val

    def emit(self, eng, fn, waits=(), sig=True):
        self.wait(eng, waits)
        ins = fn(self.engs[eng])
        if sig:
            s = self.esem[eng]
            s.count += 1
            ins.then_inc(s.h, 1)
            return Tok(s, s.count)
        return None

    def dma(self, eng, out, in_, sem, waits=(), **kw):
        self.wait(eng, waits)
        ins = self.engs[eng].dma_start(out=out, in_=in_, **kw)
        sem.count += 16
        ins.then_inc(sem.h, 16)
        return Tok(sem, sem.count)


class Ring:
    def __init__(self, bufs):
        self.bufs = bufs
        self.free = [[] for _ in bufs]
        self.i = 0

    def next(self):
        j = self.i % len(self.bufs)
        self.i += 1
        fr = self.free[j]
        self.free[j] = []
        return j, self.bufs[j], fr

    def release(self, j, toks):
        self.free[j] = list(self.free[j]) + [t for t in toks if t is not None]


class WStream:
    def __init__(self, K, nslots, name="w"):
        self.K = K
        self.n = nslots
        self.bufs = [K.sb(f"{name}{i}", [128, 16, CW], BF16) for i in range(nslots)]
        self.sems = [K.sem(f"s_{name}{i}") for i in range(nslots)]
        self.free = [[] for _ in range(nslots)]
        self.released = [True] * nslots
        self.plan_list = []
        self.next_issue = 0
        self.next_pop = 0
        self.issued = {}

    def plan(self, blocks):
        self.plan_list.extend(blocks)

    def _issue_more(self):
        while self.next_issue < len(self.plan_list):
            j = self.next_issue % self.n
            if not self.released[j]:
                break
            src_ap, nk, ncols = self.plan_list[self.next_issue]
            tok = self.K.dma("pool", self.bufs[j][:, 0:nk, 0:ncols], src_ap.rearrange("(k p) c -> p k c", p=128),
                             self.sems[j], waits=self.free[j])
            self.free[j] = []
            self.released[j] = False
            self.issued[self.next_issue] = tok
            self.next_issue += 1

    def pop(self):
        self._issue_more()
        b = self.next_pop
        self.next_pop += 1
        j = b % self.n
        return j, self.bufs[j], self.issued.pop(b)

    def release(self, j, toks):
        self.free[j] = [t for t in toks if t is not None]
        self.released[j] = True
        self._issue_more()


def rstd_from_stat(K, stat_ps, stat_tok, rstd, width, prev_readers=()):
    t1 = K.emit("act", lambda e: e.activation(out=rstd[:, 0:width], in_=stat_ps[:, 0:width], func=AF.Sqrt,
                                               bias=K.eps_t[:, 0:1], scale=1.0 / D),
                waits=[stat_tok] + list(prev_readers))
    t2 = K.emit("dve", lambda e: e.reciprocal(out=rstd[:, 0:width], in_=rstd[:, 0:width]), waits=[t1])
    return t1, t2


def post_plan(w_o, w_in, w_out):
    blocks = []
    for dg in range(D // CW):
        blocks.append((w_o[:, dg * CW:(dg + 1) * CW], 16, CW))
    for jg in range(DFF // CW):
        blocks.append((w_in[:, jg * CW:(jg + 1) * CW], 16, CW))
        blocks.append((w_in[:, DFF + jg * CW:DFF + (jg + 1) * CW], 16, CW))
    for dg in range(D // CW):
        for rb, n in enumerate([2048, 2048, 1536]):
            blocks.append((w_out[rb * 2048:rb * 2048 + n, dg * CW:(dg + 1) * CW], n // 128, CW))
    return blocks


def post_sublayers(K, half, outT, xT_dram, xTout_dram, w_o, w_in, w_out, ws, G1, A2, B2, G2, PS, st):
    nc = K.nc
    T0 = half * 512
    W = 512
    xT = st["xT"]
    ysb = st["ysb"]
    hT = st["hT"]
    aT = st["aT"]
    rstd = st["rstd"]
    tmp_ring = st["tmp_ring"]
    sq_ring = st["sq_ring"]
    sg_ring = st["sg_ring"]
    bankA = st["bankA"]
    stat_y = PS[6]
    stat_x = PS[7]

    x_ready = K.dma("sp", xT[:, :, :], xT_dram[:, T0:T0 + W].rearrange("(k p) t -> p k t", p=128), st["s_x"],
                    waits=st["xT_free"])
    st["xT_free"] = []

    def project_and_stats(nk, rhs_of_k, rhs_tok, wsrc_of_block, nblocks_rows, stat_ps, stat_free):
        y_toks = []
        last_stat = None
        nsq = 0
        for dg in range(D // CW):
            banks = []
            for dcl in range(NDC):
                bj, bank, bfree = bankA.next()
                banks.append((bj, bank, bfree))
            kk = 0
            nrb = len(nblocks_rows)
            for rb in range(nrb):
                nkb = nblocks_rows[rb]
                wj, wbuf, wtok = ws.pop()
                last = []
                for kc in range(nkb):
                    for dcl in range(NDC):
                        bj, bank, bfree = banks[dcl]
                        first = (kk == 0)
                        lastk = (kk == nk - 1)
                        t = K.emit("pe", lambda e, bank=bank, wbuf=wbuf, kc=kc, dcl=dcl, kk=kk, first=first, lastk=lastk:
                                   e.matmul(bank[:, 0:W], lhsT=wbuf[:, kc, dcl * 128:(dcl + 1) * 128], rhs=rhs_of_k(kk),
                                            start=first, stop=lastk),
                                   waits=[wtok, rhs_tok] + (bfree if first else []),
                                   sig=(lastk or kc == nkb - 1))
                        if kc == nkb - 1:
                            last.append(t)
                        if lastk:
                            banks[dcl] = (bj, bank, t)
                    kk += 1
                ws.release(wj, last)
            for dcl in range(NDC):
                dc = dg * NDC + dcl
                bj, bank, mt = banks[dcl]
                t_cp = K.emit("act", lambda e, bank=bank, dc=dc: e.activation(out=ysb[:, dc, :], in_=bank[:, 0:W], func=AF.Copy),
                              waits=[mt] + st["ysb_free"][dc])
                st["ysb_free"][dc] = []
                sj, sq, sfree = sq_ring.next()
                t_sq = K.emit("dve", lambda e, dc=dc, sq=sq: e.tensor_tensor(out=sq[:, :], in0=ysb[:, dc, :], in1=ysb[:, dc, :], op=ALU.mult),
                              waits=[t_cp] + sfree)
                bankA.release(bj, [t_cp])
                t_st = K.emit("pe", lambda e, sq=sq, nsq=nsq: e.matmul(stat_ps[:, 0:W], lhsT=K.ones_f[:, :], rhs=sq[:, :],
                                                                        start=(nsq == 0), stop=(nsq == 15)),
                              waits=[t_sq] + (stat_free if nsq == 0 else []))
                sq_ring.release(sj, [t_st])
                nsq += 1
                last_stat = t_st
                y_toks.append(t_cp)
        return y_toks, last_stat

    def residual_update(y_toks, stat_ps, stat_tok, G):
        t1, t_r = rstd_from_stat(K, stat_ps, stat_tok, rstd, W, prev_readers=st["rstd_readers"])
        st["rstd_readers"] = []
        x_toks = []
        for k in range(KD):
            tj, tmp, tfree = tmp_ring.next()
            t_a = K.emit("dve", lambda e, k=k, tmp=tmp: e.scalar_tensor_tensor(out=tmp[:, :], in0=ysb[:, k, :], scalar=G[:, k:k + 1],
                                                                                 in1=rstd[:, 0:W], op0=ALU.mult, op1=ALU.mult),
                         waits=[t_r, y_toks[k]] + tfree)
            t_b = K.emit("pool", lambda e, k=k, tmp=tmp: e.tensor_tensor(out=xT[:, k, :], in0=xT[:, k, :], in1=tmp[:, :], op=ALU.add),
                         waits=[t_a, x_ready] + st["xk_readers"][k])
            st["xk_readers"][k] = []
            tmp_ring.release(tj, [t_b])
            st["ysb_free"][k] = [t_a]
            st["rstd_readers"].append(t_a)
            x_toks.append(t_b)
        return x_toks, t1

    yt, stt = project_and_stats(KD, lambda kk: outT[:, kk, T0:T0 + W], st["outT_tok"],
                                lambda rb, dg: w_o[:, dg * CW:(dg + 1) * CW], [16], stat_y, st["stat_y_free"])
    x_toks, t_sr = residual_update(yt, stat_y, stt, G1)
    st["stat_y_free"] = [t_sr]

    last = None
    for k in range(KD):
        sj, sq, sfree = sq_ring.next()
        t_sq = K.emit("act", lambda e, k=k, sq=sq: e.activation(out=sq[:, :], in_=xT[:, k, :], func=AF.Square),
                      waits=[x_toks[k]] + sfree)
        t_st = K.emit("pe", lambda e, sq=sq, k=k: e.matmul(stat_x[:, 0:W], lhsT=K.ones_f[:, :], rhs=sq[:, :], start=(k == 0), stop=(k == KD - 1)),
                      waits=[t_sq] + (st["stat_x_free"] if k == 0 else []))
        sq_ring.release(sj, [t_st])
        st["xk_readers"][k].append(t_sq)
        last = t_st
    t1, t_r = rstd_from_stat(K, stat_x, last, rstd, W, prev_readers=st["rstd_readers"])
    st["rstd_readers"] = []
    st["stat_x_free"] = [t1]
    h_toks = []
    for k in range(KD):
        tj, tmp, tfree = tmp_ring.next()
        t_a = K.emit("dve", lambda e, k=k, tmp=tmp: e.scalar_tensor_tensor(out=tmp[:, :], in0=xT[:, k, :], scalar=A2[:, k:k + 1],
                                                                             in1=rstd[:, 0:W], op0=ALU.mult, op1=ALU.mult),
                     waits=[t_r, x_toks[k]] + tfree)
        t_b = K.emit("act", lambda e, k=k, tmp=tmp: e.activation(out=hT[:, k, :], in_=tmp[:, :], func=AF.Identity, bias=B2[:, k:k + 1], scale=1.0),
                     waits=[t_a] + st["hT_readers"])
        tmp_ring.release(tj, [t_b])
        st["xk_readers"][k].append(t_a)
        st["rstd_readers"].append(t_a)
        h_toks.append(t_b)
    st["hT_readers"] = []
    h_all = h_toks

    a_toks = [None] * NF
    for jg in range(DFF // CW):
        uj, ubuf, utok = ws.pop()
        gj, gbuf, gtok = ws.pop()
        ulast = []
        glast = []
        for jl in range(NDC):
            j = jg * NDC + jl
            ubj, ubank, ufree = bankA.next()
            gbj, gbank, gfree = bankA.next()
            for k in range(KD):
                tu = K.emit("pe", lambda e, k=k, ubank=ubank, ubuf=ubuf, jl=jl: e.matmul(ubank[:, 0:W], lhsT=ubuf[:, k, jl * 128:(jl + 1) * 128], rhs=hT[:, k, :],
                                                                                         start=(k == 0), stop=(k == KD - 1)),
                            waits=[utok] + h_all + (ufree if k == 0 else []), sig=(k == KD - 1))
            for k in range(KD):
                tg = K.emit("pe", lambda e, k=k, gbank=gbank, gbuf=gbuf, jl=jl: e.matmul(gbank[:, 0:W], lhsT=gbuf[:, k, jl * 128:(jl + 1) * 128], rhs=hT[:, k, :],
                                                                                         start=(k == 0), stop=(k == KD - 1)),
                            waits=[gtok] + (gfree if k == 0 else []), sig=(k == KD - 1))
            ulast.append(tu)
            glast.append(tg)
            sj, sg, sfree = sg_ring.next()
            t_s = K.emit("act", lambda e, gbank=gbank, sg=sg: e.activation(out=sg[:, :], in_=gbank[:, 0:W], func=AF.Silu),
                         waits=[tg] + sfree)
            t_m = K.emit("dve", lambda e, ubank=ubank, sg=sg, j=j: e.tensor_tensor(out=aT[:, j, :], in0=ubank[:, 0:W], in1=sg[:, :], op=ALU.mult),
                         waits=[tu, t_s] + st["aT_readers"][j])
            st["aT_readers"][j] = []
            sg_ring.release(sj, [t_m])
            bankA.release(ubj, [t_m])
            bankA.release(gbj, [t_s])
            a_toks[j] = t_m
        ws.release(uj, ulast)
        ws.release(gj, glast)
    st["hT_readers"] = list(ulast) + list(glast)

    yt, stt = project_and_stats(NF, lambda kk: aT[:, kk, :], a_toks,
                                lambda rb, dg: w_out[rb * 2048:rb * 2048 + [2048, 2048, 1536][rb], dg * CW:(dg + 1) * CW],
                                [16, 16, 12], stat_y, st["stat_y_free"])
    for j in range(NF):
        st["aT_readers"][j] = [stt]
    x_toks, t_sr = residual_update(yt, stat_y, stt, G2)
    st["stat_y_free"] = [t_sr]
    t_o = K.dma("sp", xTout_dram[:, T0:T0 + W].rearrange("(k p) t -> p k t", p=128), xT[:, :, :], st["s_xo"], waits=x_toks)
    st["xT_free"] = [t_o]
    return t_o


def setup_common(K):
    nc = K.nc
    K.ones_f = K.sb("ones_f", [128, 128], F32)
    K.eps_t = K.sb("eps_t", [128, 1], F32)
    K.PS = [K.es.enter_context(nc.psum_tensor(f"ps{i}", [128, 512], F32)) for i in range(8)]
    t1 = K.emit("dve", lambda e: e.memset(K.ones_f[:, :], 1.0))
    t2 = K.emit("dve", lambda e: e.memset(K.eps_t[:, :], EPS))
    K.const_tok = [t1, t2]
    for eng in ("pe", "act", "dve", "pool"):
        K.wait(eng, K.const_tok)


def make_post_state(K, nslots=4):
    st = {}
    st["xT"] = K.sb("xT_sb", [128, KD, 512], F32)
    st["ysb"] = K.sb("ysb", [128, KD, 512], F32)
    st["hT"] = K.sb("hT", [128, KD, 512], BF16)
    st["aT"] = K.sb("aT", [128, NF, 512], BF16)
    st["rstd"] = K.sb("rstd", [128, 512], F32)
    st["tmp_ring"] = Ring([K.sb(f"tmp{i}", [128, 512], F32) for i in range(2)])
    st["sq_ring"] = Ring([K.sb(f"sq{i}", [128, 512], F32) for i in range(2)])
    st["sg_ring"] = Ring([K.sb(f"sg{i}", [128, 512], F32) for i in range(2)])
    st["bankA"] = Ring(K.PS[0:6])
    st["s_x"] = K.sem("s_x")
    st["s_xo"] = K.sem("s_xo")
    st["xT_free"] = []
    st["ysb_free"] = [[] for _ in range(KD)]
    st["xk_readers"] = [[] for _ in range(KD)]
    st["rstd_readers"] = []
    st["hT_readers"] = []
    st["aT_readers"] = [[] for _ in range(NF)]
    st["stat_y_free"] = []
    st["stat_x_free"] = []
    st["ws"] = WStream(K, nslots)
    return st


def build_mod():
    nc = bass.Bass("TRN2", target_bir_lowering=False)
    cT_d = nc.dram_tensor("cT", [128, KD], F32, kind="ExternalInput").ap()
    w_d = nc.dram_tensor("adaw", [D, 6144], F32, kind="ExternalInput").ap()
    b_d = nc.dram_tensor("adab", [128, 48], F32, kind="ExternalInput").ap()
    o_d = nc.dram_tensor("modo", [128, 48], F32, kind="ExternalOutput").ap()
    with ExitStack() as es:
        K = Ctx(nc, es)
        ps = es.enter_context(nc.psum_tensor("psm", [128, 512], F32))
        cT = K.sb("cT_sb", [128, KD], F32)
        bsb = K.sb("b_sb", [128, 48], F32)
        osb = K.sb("o_sb", [128, 48], F32)
        wb = [K.sb(f"wm{i}", [128, KD, 512], F32) for i in range(3)]
        wsem = [K.sem(f"s_wm{i}") for i in range(3)]
        s_in = K.sem("s_in")
        s_out = K.sem("s_out")
        t_c = K.dma("sp", cT[:, :], cT_d, s_in)
        t_b = K.dma("sp", bsb[:, :], b_d, s_in)
        t_act = K.emit("act", lambda e: e.activation(out=cT[:, :], in_=cT[:, :], func=AF.Silu), waits=[t_b])
        ring = Ring(wb)
        last = None
        for blk in range(12):
            j, buf, fr = ring.next()
            eng = "sp" if blk % 2 == 0 else "act"
            tw = K.dma(eng, buf[:, :, :], w_d[:, blk * 512:(blk + 1) * 512].rearrange("(k p) c -> p k c", p=128), wsem[j], waits=fr)
            for cl in range(4):
                col = blk * 4 + cl
                for k in range(KD):
                    t = K.emit("pe", lambda e, buf=buf, cl=cl, k=k, col=col: e.matmul(ps[:, col:col + 1], lhsT=buf[:, k, cl * 128:(cl + 1) * 128],
                                                                                      rhs=cT[:, k:k + 1], start=(k == 0), stop=(k == KD - 1)),
                               waits=[tw, t_act], sig=(k == KD - 1 and cl == 3))
            ring.release(j, [t])
            last = t
        t_o = K.emit("dve", lambda e: e.tensor_tensor(out=osb[:, :], in0=ps[:, 0:48], in1=bsb[:, :], op=ALU.add), waits=[last, t_b])
        t_d = K.dma("sp", o_d, osb[:, :], s_out, waits=[t_o])
        K.wait("sp", [t_d])
    return nc


TWO_PI = 2.0 * np.pi


def rope_tables(K, pos_rep_d, fvec, name, scr, after=()):
    C = K.sb(name + "_C", [128, TL], F32)
    Sn = K.sb(name + "_S", [128, TL], F32)
    posi, ang, u, ui = scr["posi"], scr["ang"], scr["u"], scr["ui"]
    tp = K.dma("sp", posi[:, :], pos_rep_d, scr["sem"], waits=list(after))
    t0 = K.emit("dve", lambda e: e.tensor_copy(out=ang[:, :], in_=posi[:, :]), waits=[tp] + list(after))
    t1 = K.emit("dve", lambda e: e.tensor_scalar(out=ang[:, :], in0=ang[:, :], scalar1=fvec, scalar2=None, op0=ALU.mult), waits=[t0])
    last = t1
    toks = []
    for which, dst in ((0, Sn), (1, C)):
        off = 0.0 if which == 0 else np.pi / 2
        ta = K.emit("dve", lambda e, off=off: e.tensor_scalar(out=u[:, :], in0=ang[:, :], scalar1=float(off), scalar2=float(1.0 / TWO_PI),
                                                                op0=ALU.add, op1=ALU.mult), waits=[last])
        tb = K.emit("dve", lambda e: e.tensor_copy(out=ui[:, :], in_=u[:, :]), waits=[ta])
        tc = K.emit("dve", lambda e: e.tensor_copy(out=u[:, :], in_=ui[:, :]), waits=[tb])
        td = K.emit("dve", lambda e: e.scalar_tensor_tensor(out=u[:, :], in0=u[:, :], scalar=float(-TWO_PI), in1=ang[:, :],
                                                             op0=ALU.mult, op1=ALU.add), waits=[tc])
        te = K.emit("dve", lambda e, off=off, dst=dst: e.tensor_scalar(out=dst[:, :], in0=u[:, :], scalar1=float(off), scalar2=None, op0=ALU.add),
                    waits=[td])
        tf = K.emit("dve", lambda e, dst=dst: e.tensor_scalar(out=u[:, :], in0=dst[:, :], scalar1=float(np.pi), scalar2=float(-TWO_PI),
                                                                op0=ALU.is_ge, op1=ALU.mult), waits=[te])
        tg = K.emit("dve", lambda e, dst=dst: e.tensor_tensor(out=dst[:, :], in0=dst[:, :], in1=u[:, :], op=ALU.add), waits=[tf])
        th = K.emit("dve", lambda e, dst=dst: e.tensor_scalar(out=dst[:, :], in0=dst[:, :], scalar1=float(-3.14159), scalar2=float(3.14159),
                                                                op0=ALU.max, op1=ALU.min), waits=[tg])
        ti = K.emit("act", lambda e, dst=dst: e.activation(out=dst[:, :], in_=dst[:, :], func=AF.Sin), waits=[th])
        last = th
        toks.append(ti)
    return C, Sn, toks, last


def norm_modulate(K, xT, x_tok, A, Bv, hT, TW, sq_ring, tmp_ring, rstd, stat_banks):
    ng = TW // 512
    last = [None] * ng
    for k in range(KD):
        for g in range(ng):
            sj, sq, sfree = sq_ring.next()
            t_sq = K.emit("act", lambda e, k=k, sq=sq, g=g: e.activation(out=sq[:, :], in_=xT[:, k, g * 512:(g + 1) * 512], func=AF.Square),
                          waits=[x_tok] + sfree)
            t_st = K.emit("pe", lambda e, sq=sq, k=k, g=g: e.matmul(stat_banks[g][:, 0:512], lhsT=K.ones_f[:, :], rhs=sq[:, :],
                                                                    start=(k == 0), stop=(k == KD - 1)), waits=[t_sq])
            sq_ring.release(sj, [t_st])
            last[g] = t_st
    t_r = []
    for g in range(ng):
        t1 = K.emit("act", lambda e, g=g: e.activation(out=rstd[:, g * 512:(g + 1) * 512], in_=stat_banks[g][:, 0:512], func=AF.Sqrt,
                                                        bias=K.eps_t[:, 0:1], scale=1.0 / D), waits=[last[g]])
        t2 = K.emit("dve", lambda e, g=g: e.reciprocal(out=rstd[:, g * 512:(g + 1) * 512], in_=rstd[:, g * 512:(g + 1) * 512]), waits=[t1])
        t_r.append(t2)
    h_toks = []
    for k in range(KD):
        for g in range(ng):
            tj, tmp, tfree = tmp_ring.next()
            t_a = K.emit("dve", lambda e, k=k, tmp=tmp, g=g: e.scalar_tensor_tensor(out=tmp[:, :], in0=xT[:, k, g * 512:(g + 1) * 512], scalar=A[:, k:k + 1],
                                                                                      in1=rstd[:, g * 512:(g + 1) * 512], op0=ALU.mult, op1=ALU.mult),
                         waits=[t_r[g], x_tok] + tfree)
            t_b = K.emit("act", lambda e, k=k, tmp=tmp, g=g: e.activation(out=hT[:, k, g * 512:(g + 1) * 512], in_=tmp[:, :], func=AF.Identity,
                                                                           bias=Bv[:, k:k + 1], scale=1.0), waits=[t_a])
            tmp_ring.release(tj, [t_b])
            h_toks.append(t_b)
    return h_toks[-1]


def build_pre(kind, stage=9):
    nc = bass.Bass("TRN2", target_bir_lowering=False)
    if kind == "A":
        fm = [("q", i, 128, 0) for i in range(16)] + [("k", i, 128, 0) for i in range(4)] + [("qi", i, 128, 1) for i in range(8)] + [("ki", 0, 64, 1)]
        n_tm = 512
        ncols = 2048 + 512 + 1024 + 64 + 512 + 16
    else:
        fm = [("q", i, 128, 0) for i in range(16)] + [("k", i, 128, 0) for i in range(16)]
        n_tm = 2048
        ncols = 6144
    xT_d = nc.dram_tensor("xT", [D, TL], F32, kind="ExternalInput").ap()
    vecs_d = nc.dram_tensor("vecs", [128, 50], F32, kind="ExternalInput").ap()
    pos_d = nc.dram_tensor("posrep", [128, TL], I32, kind="ExternalInput").ap()
    w_d = nc.dram_tensor("w", [D, ncols], F32, kind="ExternalInput").ap()
    rc_d = nc.dram_tensor("rconst", [128, 256], BF16, kind="ExternalInput").ap()
    outs = {}
    outs["q"] = nc.dram_tensor("qT", [2048, TL], BF16, kind="ExternalOutput").ap()
    if kind == "A":
        outs["k"] = nc.dram_tensor("kT", [512, TL], BF16, kind="ExternalOutput").ap()
        outs["qi"] = nc.dram_tensor("qiT", [1024, TL], BF16, kind="ExternalOutput").ap()
        outs["ki"] = nc.dram_tensor("kiT", [64, TL], BF16, kind="ExternalOutput").ap()
        v_d = nc.dram_tensor("v", [TL, 512], BF16, kind="ExternalOutput").ap()
        wi_d = nc.dram_tensor("wi", [TL, 16], F32, kind="ExternalOutput").ap()
    else:
        outs["k"] = nc.dram_tensor("kT", [2048, TL], BF16, kind="ExternalOutput").ap()
        v_d = nc.dram_tensor("v", [TL, 2048], BF16, kind="ExternalOutput").ap()
        ks_d = nc.dram_tensor("ksum", [128, 16 * 32], F32, kind="ExternalOutput").ap()
    with ExitStack() as es:
        K = Ctx(nc, es)
        setup_common(K)
        PS = K.PS
        xT = K.sb("xT_sb", [128, KD, TL], F32)
        hT = K.sb("hT", [128, KD, TL], BF16)
        vecs = K.sb("vecs_sb", [128, 50], F32)
        rc = K.sb("rc_sb", [128, 256], BF16)
        rstd = K.sb("rstd", [128, TL], F32)
        sq_ring = Ring([K.sb(f"sq{i}", [128, 512], F32) for i in range(2)])
        tmp_ring = Ring([K.sb(f"tmp{i}", [128, 512], F32) for i in range(2)])
        qraw_ring = Ring([K.sb(f"qraw{i}", [128, 512], BF16) for i in range(3)])
        t2_ring = Ring([K.sb(f"t2_{i}", [128, 512], F32) for i in range(3)])
        qout_ring = Ring([K.sb(f"qout{i}", [128, TL], BF16) for i in range(3)])
        vst_ring = Ring([K.sb(f"vst{i}", [128, 256], BF16) for i in range(3)])
        scr = {"posi": K.sb("posi", [128, TL], I32), "ang": K.sb("ang", [128, TL], F32), "u": K.sb("u_s", [128, TL], F32),
               "ui": K.sb("ui", [128, TL], I32), "sem": K.sem("s_pos")}
        ws = WStream(K, 4)
        s_in = K.sem("s_in")
        s_out = K.sem("s_out")
        s_qo = [K.sem(f"s_qo{i}") for i in range(3)]
        s_vo = [K.sem(f"s_vo{i}") for i in range(3)]
        out_toks = []
        K.dma("sp", xT[:, :, :], xT_d.rearrange("(k p) t -> p k t", p=128), s_in)
        K.dma("sp", vecs[:, :], vecs_d, s_in)
        K.dma("sp", rc[:, :], rc_d, s_in)
        x_tok = Tok(s_in, s_in.count)
        for eng in ("dve", "act", "pe", "pool"):
            K.wait(eng, [x_tok])
        t_A = K.emit("dve", lambda e: e.scalar_tensor_tensor(out=vecs[:, 0:16], in0=vecs[:, 0:16], scalar=1.0, in1=vecs[:, 32:48], op0=ALU.add, op1=ALU.mult),
                     waits=[x_tok])
        K.wait("act", [t_A])
        h_tok = norm_modulate(K, xT, x_tok, vecs[:, 0:16], vecs[:, 16:32], hT, TL, sq_ring, tmp_ring, rstd, [PS[6], PS[7]])
        Cm, Sm, tk_m, lastm = rope_tables(K, pos_d, vecs[:, 48:49], "rm", scr)
        tabs = [(Cm, Sm, tk_m)]
        if kind == "A":
            Ci, Si, tk_i, lasti = rope_tables(K, pos_d, vecs[:, 49:50], "ri", scr, after=[lastm])
            tabs.append((Ci, Si, tk_i))
        if kind == "B":
            ksum = K.sb("ksum_sb", [128, 16, 32], F32)
        if stage <= 2:
            fm = []
        acc_ring = Ring(PS[0:4])
        rq_ring = Ring(PS[4:6])
        pl = []
        c_ = 0
        i_ = 0
        while i_ < len(fm):
            bw_ = sum(c[2] for c in fm[i_:i_ + NDC])
            pl.append((w_d[:, c_:c_ + bw_], KD, bw_))
            c_ += bw_
            i_ += NDC
        for c0 in range(0, n_tm if stage > 3 else 0, 256):
            pl.append((w_d[:, c_ + c0:c_ + c0 + 256], KD, 256))
        if kind == "A" and stage > 3:
            pl.append((w_d[:, c_ + n_tm:c_ + n_tm + 16], KD, 16))
        ws.plan(pl)
        col = 0
        ci = 0
        ks_toks = []
        while ci < len(fm):
            blk = fm[ci:ci + NDC]
            bw = sum(c[2] for c in blk)
            wj, wbuf, wtok = ws.pop()
            wl = []
            off = 0
            for (nm, idx, M, rk) in blk:
                Ct, St, tk = tabs[rk]
                R = rc[:, rk * 128:(rk + 1) * 128]
                banks = [acc_ring.next() for _ in range(2)]
                mt = [None, None]
                for k in range(KD):
                    for g in range(2):
                        bj, bank, bfree = banks[g]
                        mt[g] = K.emit("pe", lambda e, bank=bank, k=k, g=g, off=off, M=M, wbuf=wbuf: e.matmul(
                            bank[0:M, 0:512], lhsT=wbuf[:, k, off:off + M], rhs=hT[:, k, g * 512:(g + 1) * 512], start=(k == 0), stop=(k == KD - 1)),
                            waits=[wtok, h_tok] + (bfree if k == 0 else []), sig=(k == KD - 1))
                wl.append(mt[1])
                oj, qout, ofree = qout_ring.next()
                fin = []
                for g in range(2):
                    bj, bank, _ = banks[g]
                    rj, qraw, rfree = qraw_ring.next()
                    t_raw = K.emit("act", lambda e, bank=bank, qraw=qraw, M=M: e.activation(out=qraw[0:M, :], in_=bank[0:M, 0:512], func=AF.Copy),
                                   waits=[mt[g]] + rfree)
                    tj, t2, t2free = t2_ring.next()
                    t_t2 = K.emit("dve", lambda e, bank=bank, t2=t2, M=M, g=g, Ct=Ct: e.tensor_tensor(out=t2[0:M, :], in0=bank[0:M, 0:512],
                                                                                                      in1=Ct[0:M, g * 512:(g + 1) * 512], op=ALU.mult),
                                 waits=[mt[g], tk, t_raw] + t2free)
                    acc_ring.release(bj, [t_raw, t_t2])
                    qj, rq, rqfree = rq_ring.next()
                    t_rq = K.emit("pe", lambda e, rq=rq, qraw=qraw, M=M, R=R: e.matmul(rq[0:M, 0:512], lhsT=R[0:M, 0:M], rhs=qraw[0:M, :], start=True, stop=True),
                                  waits=[t_raw] + rqfree)
                    qraw_ring.release(rj, [t_rq])
                    mj, tmp, tfree = tmp_ring.next()
                    t_m = K.emit("dve", lambda e, rq=rq, tmp=tmp, M=M, g=g, St=St: e.tensor_tensor(out=tmp[0:M, :], in0=rq[0:M, 0:512],
                                                                                                   in1=St[0:M, g * 512:(g + 1) * 512], op=ALU.mult),
                                waits=[t_rq, tk] + tfree)
                    rq_ring.release(qj, [t_m])
                    t_f = K.emit("pool", lambda e, tmp=tmp, t2=t2, qout=qout, M=M, g=g: e.tensor_tensor(out=qout[0:M, g * 512:(g + 1) * 512], in0=tmp[0:M, :],
                                                                                                       in1=t2[0:M, :], op=ALU.add),
                                waits=[t_m, t_t2] + (ofree if g == 0 else []))
                    tmp_ring.release(mj, [t_f])
                    t2_ring.release(tj, [t_f])
                    fin.append(t_f)
                rel = []
                if kind == "B" and nm == "k":
                    t_ks = K.emit("dve", lambda e, qout=qout, idx=idx: e.tensor_reduce(out=ksum[:, idx, :], in_=qout[:, :].rearrange("p (b s) -> p b s", s=32),
                                                                                       axis=AX.X, op=ALU.add), waits=fin)
                    ks_toks.append(t_ks)
                    rel.append(t_ks)
                t_o = K.dma("sp", outs[nm][idx * 128:idx * 128 + M, :], qout[0:M, :], s_qo[oj], waits=fin)
                rel.append(t_o)
                out_toks.append(t_o)
                qout_ring.release(oj, rel)
                off += M
            ws.release(wj, wl)
            col += bw
            ci += len(blk)
        tm_blocks = [(c0, 256, "v") for c0 in range(0, n_tm, 256)]
        if stage <= 3:
            tm_blocks = []
        if kind == "A" and stage > 3:
            tm_blocks.append((n_tm, 16, "wi"))
            wist = K.sb("wist", [128, 8, 16], F32)
        for (c0, bw, nm) in tm_blocks:
            wj, wbuf, wtok = ws.pop()
            wl = []
            for tt in range(8):
                bj, bank, bfree = acc_ring.next()
                for k in range(KD):
                    t = K.emit("pe", lambda e, bank=bank, k=k, tt=tt, bw=bw, wbuf=wbuf: e.matmul(bank[:, 0:bw], lhsT=hT[:, k, tt * 128:(tt + 1) * 128],
                                                                                              rhs=wbuf[:, k, 0:bw], start=(k == 0), stop=(k == KD - 1)),
                               waits=[wtok, h_tok] + (bfree if k == 0 else []), sig=(k == KD - 1))
                wl.append(t)
                if nm == "v":
                    vj, vst, vfree = vst_ring.next()
                    t_c = K.emit("act", lambda e, bank=bank, vst=vst, bw=bw: e.activation(out=vst[:, 0:bw], in_=bank[:, 0:bw], func=AF.Copy), waits=[t] + vfree)
                    t_o = K.dma("sp", v_d[tt * 128:(tt + 1) * 128, c0:c0 + bw], vst[:, 0:bw], s_vo[vj], waits=[t_c])
                    vst_ring.release(vj, [t_o])
                    out_toks.append(t_o)
                else:
                    t_c = K.emit("act", lambda e, bank=bank, tt=tt: e.activation(out=wist[:, tt, :], in_=bank[:, 0:16], func=AF.Copy), waits=[t])
                    if tt == 7:
                        t_o = K.dma("sp", wi_d.rearrange("(a p) c -> p a c", p=128), wist[:, :, :], s_out, waits=[t_c])
                        out_toks.append(t_o)
                acc_ring.release(bj, [t_c])
            ws.release(wj, wl)
        if stage <= 2:
            K.wait("sp", tk_m)
        if kind == "B" and stage > 2:
            t_o = K.dma("sp", ks_d, ksum[:, :, :].rearrange("p a b -> p (a b)"), s_out, waits=ks_toks)
            out_toks.append(t_o)
        K.wait("sp", [Tok(s_out, s_out.count)] + [Tok(x, x.count) for x in s_qo + s_vo])
    return nc


ROPE_THETA = 500000.0


def rope_consts():
    f = np.zeros((128, 2), np.float32)
    R = np.zeros((128, 256), np.float32)
    for p in range(32):
        f[p, 0] = ROPE_THETA ** (-(p % 16) / 16.0)
    for d in range(16):
        R[d + 16, d] = -1.0
        R[d, d + 16] = 1.0
    for blk in range(2):
        b0 = blk * 64
        for p in range(16):
            f[b0 + p, 1] = ROPE_THETA ** (-(p % 8) / 8.0)
        for d in range(8):
            R[b0 + d + 8, 128 + b0 + d] = -1.0
            R[b0 + d, 128 + b0 + d + 8] = 1.0
    return f, R.astype(ml_dtypes.bfloat16)


def vec_layout(v):
    return np.ascontiguousarray(np.asarray(v).reshape(KD, 128).T)


NEG = -60000.0
SCALE = 128 ** -0.5


def barrier(K):
    toks = [Tok(s, s.count) for s in K.esem.values() if s.count > 0]
    for eng in ("pe", "act", "dve", "pool", "sp"):
        K.wait(eng, toks)


def attention_core(K, heads, tgs, get_kv, qT_of_head, bias_mm, outT, cst, st_banks, acc_pairs):
    st_ring = Ring(st_banks)
    acc_ring = Ring(acc_pairs)
    pT_ring = cst["pT_ring"]
    rden_ring = cst["rden_ring"]
    for h in heads:
        kT, vt, kv_tok, kv_release = get_kv(h)
        qf, q_tok = qT_of_head(h)
        last_pe = None
        for tg in tgs:
            qt0 = 4 * tg
            aj, (oacc, dacc), afree = acc_ring.next()
            first = True
            nm = qt0 + 4
            for m in range(nm):
                c0 = 128 * max(0, m - qt0)
                N = 512 - c0
                for r in range(8):
                    lastt = (m == nm - 1 and r == 7)
                    sj, st, sfree = st_ring.next()
                    extra = bias_mm(h, tg, m, r, c0)
                    K.emit("pe", lambda e, st=st, kT=kT, r=r, m=m, c0=c0, tg=tg: e.matmul(
                        st[:, c0:512], lhsT=kT[:, r, m * 128:(m + 1) * 128], rhs=qf(tg * 512 + c0, tg * 512 + 512), start=True, stop=False),
                        waits=[kv_tok, q_tok] + sfree, sig=False)
                    t_s = None
                    for xi, (fn, xw) in enumerate(extra):
                        t_s = K.emit("pe", lambda e, fn=fn, st=st: fn(e, st), waits=xw, sig=(xi == len(extra) - 1))
                    pj, pT, pfree = pT_ring.next()
                    t_e = K.emit("act", lambda e, st=st, pT=pT, c0=c0, N=N: e.activation(out=pT[:, 0:N], in_=st[:, c0:512], func=AF.Exp, scale=SCALE),
                                 waits=[t_s] + pfree)
                    st_ring.release(sj, [t_e])
                    K.emit("pe", lambda e, oacc=oacc, vt=vt, r=r, m=m, pT=pT, c0=c0, N=N, first=first, lastt=lastt: e.matmul(
                        oacc[:, c0:512], lhsT=vt[:, r * 8 + m, :], rhs=pT[:, 0:N], start=first, stop=lastt),
                        waits=[t_e] + (afree if first else []), sig=False)
                    t_p = K.emit("pe", lambda e, dacc=dacc, pT=pT, c0=c0, N=N, first=first, lastt=lastt: e.matmul(
                        dacc[:, c0:512], lhsT=cst["ones_bf"][:, :], rhs=pT[:, 0:N], start=first, stop=lastt), sig=True)
                    pT_ring.release(pj, [t_p])
                    first = False
                    last_pe = t_p
            rj, rden, rfree = rden_ring.next()
            t_r = K.emit("dve", lambda e, rden=rden, dacc=dacc: e.reciprocal(out=rden[:, :], in_=dacc[:, 0:512]), waits=[last_pe] + rfree)
            t_o = K.emit("dve", lambda e, rden=rden, oacc=oacc, h=h, tg=tg: e.tensor_tensor(out=outT[:, h, tg * 512:(tg + 1) * 512], in0=oacc[:, 0:512],
                                                                                           in1=rden[:, :], op=ALU.mult), waits=[t_r])
            rden_ring.release(rj, [t_o])
            acc_ring.release(aj, [t_o])
            cst["out_tok"] = t_o
        kv_release(h, [last_pe])


def att_consts(core):
    cb = np.zeros((128, 128 + 128 + 1024 + 1024 + 1024), np.float32)
    cb[:, 0:128] = np.eye(128)
    cb[:, 128:256] = 1.0
    for m in range(8):
        for p in range(128):
            cb[4 * m + p // 32, 256 + m * 128 + p] = 1.0
    p = np.arange(128)[:, None]
    t = np.arange(128)[None, :]
    for r in range(8):
        ok = (p < t) | ((p == t) & (r <= core))
        same = (p // 32) == (t // 32)
        cb[:, 1280 + r * 128:1280 + (r + 1) * 128] = np.where(same & ~ok, NEG, 0.0)
        cb[:, 2304 + r * 128:2304 + (r + 1) * 128] = np.where(ok.T, 0.0, -1e30)
    cf = np.zeros((128, 128 + 256 + 256 + 256), np.float32)
    cf[:, 0:128] = np.eye(128)
    for qt in range(8):
        j = 4 * qt + np.arange(128) // 32
        b = np.arange(32)[None, :]
        pv = (b < j[:, None]).astype(np.float32)
        cf[:, 128 + qt * 32:128 + (qt + 1) * 32] = pv
        cf[:, 384 + qt * 32:384 + (qt + 1) * 32] = (b == j[:, None]).astype(np.float32)
        cf[:, 640 + qt * 32:640 + (qt + 1) * 32] = (pv - 1.0) * 1e30
    return cb.astype(ml_dtypes.bfloat16), cf


def build_att(kind, with_post=True):
    nc = bass.Bass("TRN2", target_bir_lowering=False)
    nkv = 4 if kind == "A" else 16
    qT_d = nc.dram_tensor("qT", [D, TL], BF16, kind="ExternalInput").ap()
    kT_d = nc.dram_tensor("kTall", [nkv, 128, 8, TL], BF16, kind="ExternalInput").ap()
    v_d = nc.dram_tensor("vall", [nkv, 128, 64, 128], BF16, kind="ExternalInput").ap()
    cb_d = nc.dram_tensor("cb", [128, 3328], BF16, kind="ExternalInput").ap()
    cf_d = nc.dram_tensor("cf", [128, 896], F32, kind="ExternalInput").ap()
    vecs_d = nc.dram_tensor("vecs", [128, 112], F32, kind="ExternalInput").ap()
    xT_d = nc.dram_tensor("xT", [D, TL], F32, kind="ExternalInput").ap()
    w_o = nc.dram_tensor("w_o", [D, D], F32, kind="ExternalInput").ap()
    w_in = nc.dram_tensor("w_in", [D, 2 * DFF], F32, kind="ExternalInput").ap()
    w_out = nc.dram_tensor("w_out", [DFF, D], F32, kind="ExternalInput").ap()
    if kind == "B":
        ks_d = nc.dram_tensor("ksall", [128, 8, 512], F32, kind="ExternalInput").ap()
    else:
        qi_d = nc.dram_tensor("qiT", [1024, TL], BF16, kind="ExternalInput").ap()
        ki_d = nc.dram_tensor("kiall", [64, 8, 8, 128], BF16, kind="ExternalInput").ap()
        wi_d = nc.dram_tensor("wi", [TL, 16], F32, kind="ExternalInput").ap()
    xo_d = nc.dram_tensor("xo", [D, TL], F32, kind="ExternalOutput").ap()
    if not with_post:
        ao_d = nc.dram_tensor("ao", [D, TL], BF16, kind="ExternalOutput").ap()
    with ExitStack() as es:
        K = Ctx(nc, es)
        setup_common(K)
        PS = K.PS
        outT = K.sb("outT_sb", [128, KD, TL], BF16)
        vecs = K.sb("vecs_sb", [128, 112], F32)
        s_in = K.sem("s_in")
        s_vec = K.sem("s_vec")
        t_vl = K.dma("sp", vecs[:, :], vecs_d, s_vec)
        t_v1 = K.emit("dve", lambda e: e.tensor_tensor(out=vecs[:, 0:16], in0=vecs[:, 0:16], in1=vecs[:, 64:80], op=ALU.mult), waits=[t_vl])
        t_v2 = K.emit("dve", lambda e: e.scalar_tensor_tensor(out=vecs[:, 16:32], in0=vecs[:, 16:32], scalar=1.0, in1=vecs[:, 80:96], op0=ALU.add, op1=ALU.mult), waits=[t_v1])
        t_v3 = K.emit("dve", lambda e: e.tensor_tensor(out=vecs[:, 48:64], in0=vecs[:, 48:64], in1=vecs[:, 96:112], op=ALU.mult), waits=[t_v2])
        for eng in ("act", "dve", "pool", "pe"):
            K.wait(eng, [t_v3])
        with ExitStack() as es2:
            K2 = K
            old_es = K.es
            K.es = es2
            cb = K.sb("cb_sb", [128, 3328], BF16)
            cf = K.sb("cf_sb", [128, 896], F32)
            K.dma("sp", cb[:, :], cb_d, s_in)
            K.dma("sp", cf[:, :], cf_d, s_in)
            cst = {}
            if kind == "B":
                qT = K.sb("qT_sb", [128, KD, TL], BF16)
                K.dma("sp", qT[:, :, :], qT_d.rearrange("(k p) t -> p k t", p=128), s_in)
                cst = {"ones_bf": cb[:, 128:256],
                       "pT_ring": Ring([K.sb(f"pT{i}", [128, 512], BF16) for i in range(4)]),
                       "rden_ring": Ring([K.sb(f"rden{i}", [128, 512], F32) for i in range(2)])}
                kbuf = [K.sb(f"kTb{i}", [128, 8, TL], BF16) for i in range(2)]
                vbuf = [K.sb(f"vb{i}", [128, 64, 128], BF16) for i in range(2)]
                ksem = [K.sem(f"s_kv{i}") for i in range(2)]
            in_tok = Tok(s_in, s_in.count)
            for eng in ("pe", "act", "dve", "pool"):
                K.wait(eng, [in_tok])
            ident_bf = cb[:, 0:128]
            ident_f = cf[:, 0:128]
            kvfree = [[], []]
            kv_loaded = {}

            def load_kv(g):
                j = g % 2
                K.dma("sp", kbuf[j][:, :, :], kT_d[g], ksem[j], waits=kvfree[j])
                t = K.dma("sp", vbuf[j][:, :, :], v_d[g], ksem[j], waits=kvfree[j])
                kvfree[j] = []
                kv_loaded[g] = t

            if kind == "B":
                ksa = K.sb("ksa", [128, 8, 512], F32)
                kmean = K.sb("kmean", [128, 512], BF16)
                gm = K.sb("gm", [128, 256], F32)
                top8 = K.sb("top8", [128, 8, 8], F32)
                sel = K.sb("sel", [128, 256], F32)
                selbT = [K.sb(f"selbT{i}", [32, TL], BF16) for i in range(2)]
                selb_free = [[], []]
                s_ks = K.sem("s_ks")
                t_ks = K.dma("sp", ksa[:, :, :], ks_d, s_ks)
                t = t_ks
                for r in range(1, 8):
                    t = K.emit("dve", lambda e, r=r: e.tensor_tensor(out=ksa[:, 0, :], in0=ksa[:, 0, :], in1=ksa[:, r, :], op=ALU.add), waits=[t])
                t_km = K.emit("dve", lambda e: e.tensor_scalar(out=kmean[:, :], in0=ksa[:, 0, :], scalar1=1.0 / 256, scalar2=None, op0=ALU.mult), waits=[t])
                load_kv(0)
                gate_state = {"bank_free": [], "sel_tok": {}}

                def moba_select(h):
                    gps = PS[3]
                    j = h % 2
                    tg_ = None
                    for qt in range(8):
                        tg_ = K.emit("pe", lambda e, qt=qt: e.matmul(gps[:, qt * 32:(qt + 1) * 32], lhsT=qT[:, h, qt * 128:(qt + 1) * 128],
                                                                      rhs=kmean[:, h * 32:(h + 1) * 32], start=True, stop=True),
                                     waits=[t_km] + (gate_state["bank_free"] if qt == 0 else []), sig=(qt == 7))
                    t1 = K.emit("dve", lambda e: e.tensor_tensor(out=gm[:, :], in0=gps[:, 0:256], in1=cf[:, 640:896], op=ALU.add), waits=[tg_])
                    tt = t1
                    for qt in range(8):
                        tt = K.emit("dve", lambda e, qt=qt: e.max(out=top8[:, qt, :], in_=gm[:, qt * 32:(qt + 1) * 32]), waits=[tt])
                    for qt in range(8):
                        tt = K.emit("dve", lambda e, qt=qt: e.tensor_scalar(out=sel[:, qt * 32:(qt + 1) * 32], in0=gm[:, qt * 32:(qt + 1) * 32],
                                                                              scalar1=top8[:, qt, 2:3], scalar2=None, op0=ALU.is_ge), waits=[tt])
                    tt = K.emit("dve", lambda e: e.tensor_tensor(out=sel[:, :], in0=sel[:, :], in1=cf[:, 128:384], op=ALU.mult), waits=[tt])
                    tt = K.emit("dve", lambda e: e.tensor_tensor(out=sel[:, :], in0=sel[:, :], in1=cf[:, 384:640], op=ALU.add), waits=[tt])
                    tt = K.emit("dve", lambda e: e.tensor_scalar(out=sel[:, :], in0=sel[:, :], scalar1=-1.0, scalar2=-NEG, op0=ALU.add, op1=ALU.mult), waits=[tt])
                    tlast = None
                    for half in range(2):
                        tp = None
                        for q4 in range(4):
                            qt = half * 4 + q4
                            tp = K.emit("pe", lambda e, qt=qt, q4=q4: e.transpose(out=gps[0:32, q4 * 128:(q4 + 1) * 128], in_=sel[:, qt * 32:(qt + 1) * 32],
                                                                                identity=ident_f),
                                        waits=[tt] + ([t1] if half == 0 and q4 == 0 else []) + ([tlast] if half == 1 and q4 == 0 else []), sig=(q4 == 3))
                        tlast = K.emit("act", lambda e, half=half: e.activation(out=selbT[j][:, half * 512:(half + 1) * 512], in_=gps[0:32, 0:512], func=AF.Copy),
                                       waits=[tp] + selb_free[j])
                    selb_free[j] = []
                    gate_state["bank_free"] = [tlast]
                    gate_state["sel_tok"][h] = tlast

                def get_kv(h):
                    if h + 1 < 16:
                        load_kv(h + 1)
                    moba_select(h)
                    j = h % 2

                    def rel(hh, toks):
                        kvfree[hh % 2] = list(toks)
                        selb_free[hh % 2] = list(toks)
                    return kbuf[j], vbuf[j], kv_loaded[h], rel

                def qf_of(h):
                    return (lambda lo, hi: qT[:, h, lo:hi]), in_tok

                def bias_mm(h, tg, m, r, c0):
                    j = h % 2
                    stk = gate_state["sel_tok"][h]
                    lst = []
                    qt0 = 4 * tg
                    has_diag = (m >= qt0)
                    lst.append((lambda e, st, m=m, c0=c0, tg=tg, j=j, has_diag=has_diag: e.matmul(
                        st[:, c0:512], lhsT=cb[0:32, 256 + m * 128:256 + (m + 1) * 128], rhs=selbT[j][:, tg * 512 + c0:tg * 512 + 512],
                        start=False, stop=(not has_diag)), [stk]))
                    if has_diag:
                        lst.append((lambda e, st, r=r, c0=c0: e.matmul(st[:, c0:c0 + 128], lhsT=ident_bf, rhs=cb[:, 1280 + r * 128:1280 + (r + 1) * 128],
                                                                      start=False, stop=True), []))
                    return lst

                attention_core(K, list(range(16)), [0, 1], get_kv, qf_of, bias_mm, outT, cst, PS[0:3], [(PS[4], PS[5]), (PS[6], PS[7])])

            if kind == "A":
                wi_sb = K.sb("wi_sb", [128, 8, 16], F32)
                maskbT = K.sb("maskbT", [128, 208, 128], BF16)
                sm = K.sb("sm", [128, 8], F32)
                s_a = K.sem("s_a")
                K.dma("sp", wi_sb[:, :, :], wi_d.rearrange("(a p) c -> p a c", p=128), s_a)
                for tg in range(2):
                    qt0 = 4 * tg
                    offs = {}
                    o_ = 0
                    for qt in range(qt0, qt0 + 4):
                        offs[qt] = o_
                        o_ += 8 * (qt + 1)
                    with ExitStack() as es3:
                        K.es = es3
                        qiT = K.sb("qiT_sb_" + str(tg), [128, 8, 512], BF16)
                        kiT = K.sb("kiT_sb_" + str(tg), [128, 8, 8, 128], BF16)
                        isc = K.sb("isc_" + str(tg), [128, 8192], F32)
                        junk = K.sb("junk_" + str(tg), [128, 8192], BF16)
                        diag = K.sb("diag_" + str(tg), [128, 16, 128], BF16)
                        rl_ring = Ring([K.sb(f"rl{i}_{tg}", [128, 512], BF16) for i in range(4)])
                        K.dma("sp", qiT[:, :, :], qi_d[:, tg * 512:(tg + 1) * 512].rearrange("(k p) t -> p k t", p=128), s_a)
                        K.dma("sp", kiT[0:64, :, :, :], ki_d, s_a)
                        K.dma("sp", kiT[64:128, :, :, :], ki_d, s_a)
                        a_tok = Tok(s_a, s_a.count)
                        for eng in ("pe", "act", "dve"):
                            K.wait(eng, [a_tok])
                        d_ring = Ring(PS[0:3])
                        i_ring = Ring(PS[3:5])
                        t_ring = Ring(PS[5:7])
                        chain = None
                        for qt in range(qt0, qt0 + 4):
                            tl = (qt - qt0) * 128
                            ncol = 1024 * (qt + 1)
                            td = None
                            for hh in range(16):
                                td = K.emit("dve", lambda e, hh=hh, qt=qt: e.tensor_scalar(out=diag[:, hh, :], in0=ident_bf, scalar1=wi_sb[:, qt, hh:hh + 1],
                                                                                            scalar2=None, op0=ALU.mult), waits=[chain, cst.get("diag_free")])
                            ev_toks = []
                            for grp in range(2 * (qt + 1)):
                                m = grp // 2
                                r0 = (grp % 2) * 4
                                ij, ips, ifree = i_ring.next()
                                ti = None
                                for hh in range(16):
                                    hp = hh // 2
                                    po = (hh % 2) * 64
                                    dj, dps, dfree = d_ring.next()
                                    t_d = K.emit("pe", lambda e, dps=dps, po=po, hp=hp, tl=tl, m=m, r0=r0: e.matmul(
                                        dps[:, 0:512], lhsT=qiT[po:po + 64, hp, tl:tl + 128], rhs=kiT[po:po + 64, m, r0:r0 + 4, :], start=True, stop=True),
                                        waits=dfree)
                                    rj, rl, rfree = rl_ring.next()
                                    t_r = K.emit("act", lambda e, dps=dps, rl=rl: e.activation(out=rl[:, :], in_=dps[:, 0:512], func=AF.Relu), waits=[t_d] + rfree)
                                    d_ring.release(dj, [t_r])
                                    ti = K.emit("pe", lambda e, ips=ips, hh=hh, rl=rl: e.matmul(ips[:, 0:512], lhsT=diag[:, hh, :], rhs=rl[:, :],
                                                                                              start=(hh == 0), stop=(hh == 15)),
                                                waits=[t_r, td] + (ifree if hh == 0 else []))
                                    rl_ring.release(rj, [ti])
                                t_ev = K.emit("act", lambda e, ips=ips, grp=grp: e.activation(out=isc[:, grp * 512:(grp + 1) * 512], in_=ips[:, 0:512], func=AF.Copy),
                                              waits=[ti, chain])
                                i_ring.release(ij, [t_ev])
                                ev_toks.append(t_ev)
                            cst["diag_free"] = ti
                            t = K.emit("dve", lambda e, ncol=ncol: e.tensor_reduce(out=sm[:, 1:2], in_=isc[:, 0:ncol], axis=AX.X, op=ALU.max), waits=ev_toks + [chain])
                            t = K.emit("dve", lambda e, ncol=ncol: e.tensor_reduce(out=sm[:, 0:1], in_=isc[:, 0:ncol], axis=AX.X, op=ALU.min), waits=[t])
                            for r in range(8):
                                c_ = qt * 1024 + r * 128
                                t = K.emit("dve", lambda e, c_=c_, r=r: e.tensor_tensor(out=isc[:, c_:c_ + 128], in0=isc[:, c_:c_ + 128],
                                                                                         in1=cb[:, 2304 + r * 128:2304 + (r + 1) * 128], op=ALU.add), waits=[t])
                            for it in range(22):
                                t = K.emit("dve", lambda e: e.tensor_scalar(out=sm[:, 2:3], in0=sm[:, 0:1], scalar1=sm[:, 1:2], scalar2=0.5, op0=ALU.add, op1=ALU.mult), waits=[t])
                                t = K.emit("dve", lambda e, ncol=ncol: e.tensor_scalar(out=junk[:, 0:ncol], in0=isc[:, 0:ncol], scalar1=sm[:, 2:3], scalar2=None,
                                                                                        op0=ALU.is_ge, op1=ALU.add, accum_out=sm[:, 3:4]), waits=[t])
                                t = K.emit("dve", lambda e: e.tensor_scalar(out=sm[:, 4:5], in0=sm[:, 3:4], scalar1=255.5, scalar2=None, op0=ALU.is_ge), waits=[t])
                                t = K.emit("dve", lambda e: e.tensor_tensor(out=sm[:, 5:6], in0=sm[:, 2:3], in1=sm[:, 0:1], op=ALU.subtract), waits=[t])
                                t = K.emit("dve", lambda e: e.tensor_tensor(out=sm[:, 6:7], in0=sm[:, 4:5], in1=sm[:, 5:6], op=ALU.mult), waits=[t])
                                t = K.emit("dve", lambda e: e.tensor_tensor(out=sm[:, 0:1], in0=sm[:, 0:1], in1=sm[:, 6:7], op=ALU.add), waits=[t])
                                t = K.emit("dve", lambda e: e.tensor_tensor(out=sm[:, 1:2], in0=sm[:, 2:3], in1=sm[:, 6:7], op=ALU.add), waits=[t])
                            t = K.emit("dve", lambda e, ncol=ncol: e.tensor_scalar(out=isc[:, 0:ncol], in0=isc[:, 0:ncol], scalar1=sm[:, 0:1], scalar2=-1.0,
                                                                                    op0=ALU.is_ge, op1=ALU.add), waits=[t])
                            ntile = 8 * (qt + 1)
                            t_last = None
                            for b4 in range(ntile // 4):
                                tj, tps, tfree = t_ring.next()
                                tp = None
                                for q4 in range(4):
                                    ti_ = b4 * 4 + q4
                                    tp = K.emit("pe", lambda e, tps=tps, q4=q4, ti_=ti_: e.transpose(out=tps[:, q4 * 128:(q4 + 1) * 128], in_=isc[:, ti_ * 128:(ti_ + 1) * 128],
                                                                                                    identity=ident_f), waits=[t] + (tfree if q4 == 0 else []), sig=(q4 == 3))
                                base = offs[qt] + b4 * 4
                                t_last = K.emit("act", lambda e, tps=tps, base=base: e.activation(
                                    out=maskbT[:, base:base + 4, :], in_=tps[:, 0:512].rearrange("p (a b) -> p a b", b=128), func=AF.Copy, scale=-NEG),
                                    waits=[tp, cst.get("mask_free")])
                                t_ring.release(tj, [t_last])
                            chain = K.emit("dve", lambda e: e.memset(sm[:, 7:8], 0.0), waits=[t_last, t])
                        barrier(K)
                        K.es = es2
                    with ExitStack() as es4:
                        K.es = es4
                        pcst = {"ones_bf": cb[:, 128:256],
                                "pT_ring": Ring([K.sb(f"pT{i}_{tg}", [128, 512], BF16) for i in range(4)]),
                                "rden_ring": Ring([K.sb(f"rden{i}_{tg}", [128, 512], F32) for i in range(2)])}
                        qT = K.sb("qT_sb_" + str(tg), [128, KD, TL], BF16)
                        s_q = K.sem(f"s_q{tg}")
                        q_tok = K.dma("sp", qT[:, :, :], qT_d.rearrange("(k p) t -> p k t", p=128), s_q)
                        kbuf = [K.sb(f"kTb{i}_{tg}", [128, 8, TL], BF16) for i in range(2)]
                        vbuf = [K.sb(f"vb{i}_{tg}", [128, 64, 128], BF16) for i in range(2)]
                        ksem = [K.sem(f"s_kv{i}_{tg}") for i in range(2)]
                        kvfree = [[], []]
                        kv_loaded = {}

                        def load_kv(g, kbuf=kbuf, vbuf=vbuf, ksem=ksem, kvfree=kvfree, kv_loaded=kv_loaded):
                            j = g % 2
                            K.dma("sp", kbuf[j][:, :, :], kT_d[g], ksem[j], waits=kvfree[j])
                            kv_loaded[g] = K.dma("sp", vbuf[j][:, :, :], v_d[g], ksem[j], waits=kvfree[j])
                            kvfree[j] = []

                        load_kv(0)

                        def get_kv(h, kbuf=kbuf, vbuf=vbuf, kvfree=kvfree, kv_loaded=kv_loaded, load_kv=load_kv):
                            g = h // 4
                            if h % 4 == 0 and g + 1 < 4:
                                load_kv(g + 1)

                            def rel(hh, toks):
                                if hh % 4 == 3:
                                    kvfree[(hh // 4) % 2] = list(toks)
                            return kbuf[g % 2], vbuf[g % 2], kv_loaded[g], rel

                        def qf_of(h, qT=qT, q_tok=q_tok):
                            return (lambda lo, hi: qT[:, h, lo:hi]), q_tok

                        def bias_mm(h, tg_, m, r, c0, offs=offs, qt0=qt0):
                            lst = []
                            for qt in range(max(m, qt0), qt0 + 4):
                                cc = (qt - qt0) * 128
                                lst.append((lambda e, st, cc=cc, qt=qt, m=m, r=r: e.matmul(st[:, cc:cc + 128], lhsT=ident_bf, rhs=maskbT[:, offs[qt] + m * 8 + r, :],
                                                                                         start=False, stop=(qt == qt0 + 3)), []))
                            return lst

                        attention_core(K, list(range(16)), [tg], get_kv, qf_of, bias_mm, outT, pcst, PS[0:3], [(PS[3], PS[4]), (PS[5], PS[6])])
                        cst["out_tok"] = pcst["out_tok"]
                        cst["mask_free"] = pcst["out_tok"]
                        barrier(K)
                        K.es = es2
            barrier(K)
            K.es = old_es
        st = None
        if with_post:
            K.wait("pe", [in_tok])
            st = make_post_state(K)
            st["outT_tok"] = cst["out_tok"]
            st["ws"].plan(post_plan(w_o, w_in, w_out) * 2)
            last = None
            for half in range(2):
                last = post_sublayers(K, half, outT, xT_d, xo_d, w_o, w_in, w_out, st["ws"],
                                      vecs[:, 0:16], vecs[:, 16:32], vecs[:, 32:48], vecs[:, 48:64], PS, st)
            K.wait("sp", [last])
        else:
            s_o = K.sem("s_o")
            t_o = K.dma("sp", ao_d.rearrange("(k p) t -> p k t", p=128), outT[:, :, :], s_o, waits=[cst["out_tok"]])
            K.wait("sp", [t_o])
    return nc


_PROGS = {}


def _prog(name, fn):
    if name not in _PROGS:
        _PROGS[name] = fn()
    return _PROGS[name]


def _run(nc, in_maps):
    res = run_bass_kernel_spmd(nc, in_maps, core_ids=list(range(NCORES)))
    return res.results


def kernel(x, c, positions, a_w_in, a_w_o, b_w_in, b_w_o, ada_w, ada_b, norm_g, ffn_w_in, ffn_w_out):
    x = np.asarray(x, np.float32)
    c = np.asarray(c, np.float32)
    positions = np.asarray(positions, np.int32)
    a_w_in, a_w_o, b_w_in, b_w_o = [np.asarray(a, np.float32) for a in (a_w_in, a_w_o, b_w_in, b_w_o)]
    ada_w, ada_b, norm_g = [np.asarray(a, np.float32) for a in (ada_w, ada_b, norm_g)]
    ffn_w_in, ffn_w_out = np.asarray(ffn_w_in, np.float32), np.asarray(ffn_w_out, np.float32)
    C = NCORES
    cT = vec_layout(c[0])
    maps = []
    for r in range(C):
        i, hf = r // 2, r % 2
        maps.append({"cT": cT, "adaw": np.ascontiguousarray(ada_w[i][:, hf * 6144:(hf + 1) * 6144]),
                     "adab": np.ascontiguousarray(ada_b[i][hf * 6144:(hf + 1) * 6144].reshape(48, 128).T)})
    mo = _run(_prog("mod", build_mod), maps)
    mod = np.zeros((4, 6, 128, 16), np.float32)
    for i in range(4):
        for ch in range(96):
            mod[i, ch // 16, :, ch % 16] = np.asarray(mo[2 * i + ch // 48]["modo"])[:, ch % 48]
    fvec, R = rope_consts()
    xT = [np.ascontiguousarray(x[0, r::C, :].T) for r in range(C)]
    posrep = [np.ascontiguousarray(np.broadcast_to(positions[0, r::C][None, :], (128, TL))) for r in range(C)]
    consts = [att_consts(r) for r in range(C)]
    for i in range(4):
        kind = "A" if i % 2 == 0 else "B"
        sh1, sc1, g1, sh2, sc2, g2 = [mod[i, j] for j in range(6)]
        ng = [vec_layout(norm_g[i, j]) for j in range(4)]
        if kind == "A":
            w = a_w_in[i // 2]
            cuts = np.cumsum([2048, 512, 512, 1024, 64])
            wq, wk, wv, wqi, wki, wwi = np.split(w, cuts, axis=1)
            wperm = np.ascontiguousarray(np.concatenate([wq, wk, wqi, wki, wv, wwi], axis=1))
            w_o = a_w_o[i // 2]
            nkv = 4
        else:
            wperm = np.ascontiguousarray(b_w_in[i // 2])
            w_o = b_w_o[i // 2]
            nkv = 16
        vecs_pre = np.ascontiguousarray(np.concatenate([sc1, sh1, ng[0], fvec], axis=1).astype(np.float32))
        maps = [{"xT": xT[r], "vecs": vecs_pre, "posrep": posrep[r], "w": wperm, "rconst": R} for r in range(C)]
        pr = _run(_prog("pre" + kind, lambda: build_pre(kind)), maps)
        kT = np.stack([np.asarray(pr[r]["kT"]) for r in range(C)])
        kTall = np.ascontiguousarray(kT.reshape(C, nkv, 128, TL).transpose(1, 2, 0, 3))
        vv = np.stack([np.asarray(pr[r]["v"]) for r in range(C)])
        vall = np.ascontiguousarray(vv.reshape(C, 8, 128, nkv, 128).transpose(3, 2, 0, 1, 4).reshape(nkv, 128, 64, 128))
        vecs_att = np.ascontiguousarray(np.concatenate([g1, sc2, sh2, g2, ng[1], ng[2], ng[3]], axis=1).astype(np.float32))
        maps = []
        for r in range(C):
            m_ = {"qT": np.asarray(pr[r]["qT"]), "kTall": kTall, "vall": vall, "cb": consts[r][0], "cf": consts[r][1], "vecs": vecs_att,
                  "xT": xT[r], "w_o": w_o, "w_in": ffn_w_in[i], "w_out": ffn_w_out[i]}
            maps.append(m_)
        if kind == "A":
            kiT = np.stack([np.asarray(pr[r]["kiT"]) for r in range(C)])
            kiall = np.ascontiguousarray(kiT.reshape(C, 64, 8, 128).transpose(1, 2, 0, 3))
            for r in range(C):
                maps[r]["qiT"] = np.asarray(pr[r]["qiT"])
                maps[r]["kiall"] = kiall
                maps[r]["wi"] = np.asarray(pr[r]["wi"])
        else:
            ks = np.stack([np.asarray(pr[r]["ksum"]) for r in range(C)])
            ksall = np.ascontiguousarray(ks.transpose(1, 0, 2))
            for r in range(C):
                maps[r]["ksall"] = ksall
        ar = _run(_prog("att" + kind, lambda: build_att(kind)), maps)
        xT = [np.asarray(ar[r]["xo"]) for r in range(C)]
    out = np.zeros((1, S, D), np.float32)
    for r in range(C):
        out[0, r::C, :] = xT[r].T
    return out
```

```python
import numpy as np
import ml_dtypes
from contextlib import ExitStack
import concourse.bass as bass
import concourse.mybir as mybir
from concourse.bass_utils import run_bass_kernel_spmd

F32 = mybir.dt.float32
BF16 = mybir.dt.bfloat16
I32 = mybir.dt.int32
U8 = mybir.dt.uint8
AF = mybir.ActivationFunctionType
ALU = mybir.AluOpType
AX = mybir.AxisListType

NCORES = 8
D = 2048
KD = 16
S = 8192
TL = 1024
DFF = 5632
NF = 44
EPS = 1e-6
CW = 256
NDC = CW // 128


class SemC:
    def __init__(self, K, name):
        self.K = K
        self.name = name
        self.epoch = -1
        self._h = None
        self._count = 0

    def touch(self):
        if self.epoch != self.K.epoch:
            self.epoch = self.K.epoch
            self._h = self.K.epoch_es.enter_context(self.K.nc.semaphore(f"{self.K.pfx}{self.name}_e{self.epoch}"))
            self._count = 0

    @property
    def h(self):
        self.touch()
        return self._h

    @property
    def count(self):
        self.touch()
        return self._count

    @count.setter
    def count(self, v):
        self.touch()
        self._count = v


class Tok:
    __slots__ = ("sem", "val", "epoch")

    def __init__(self, sem, val):
        self.sem = sem
        self.val = val
        self.epoch = sem.K.epoch


class Ctx:
    ENG = ("pe", "act", "dve", "pool", "sp")

    def __init__(self, nc, es, pfx=""):
        self.nc = nc
        self.es = es
        self.pfx = pfx
        self.epoch = 0
        self.epoch_es = ExitStack()
        self.esem = {n: SemC(self, "s_" + n) for n in ("pe", "act", "dve", "pool")}
        self.waited = {}
        self.rec = {n: [] for n in self.ENG}

    def sem(self, name):
        return SemC(self, name)

    def sb(self, name, shape, dt):
        return self.es.enter_context(self.nc.sbuf_tensor(self.pfx + name, shape, dt))

    def _flat(self, toks, acc):
        for t in toks:
            if t is None:
                continue
            if isinstance(t, (list, tuple)):
                self._flat(t, acc)
            else:
                if t.epoch != self.epoch:
                    continue
                k = id(t.sem)
                if k not in acc or acc[k].val < t.val:
                    acc[k] = t
        return acc

    def wait(self, eng, toks):
        for t in self._flat(toks, {}).values():
            key = (eng, id(t.sem))
            if self.waited.get(key, 0) >= t.val:
                continue
            self.rec[eng].append(("w", t.sem, t.val))
            self.waited[key] = t.val

    def emit(self, eng, fn, waits=(), sig=True):
        self.wait(eng, waits)
        calls = []

        class _P:
            def __getattr__(self_, name):
                def f(*a, **kw):
                    calls.append((name, a, kw))
                    return None
                return f
        fn(_P())
        assert len(calls) == 1
        name_, a_, kw_ = calls[0]
        fn = lambda e, name_=name_, a_=a_, kw_=kw_: getattr(e, name_)(*a_, **kw_)
        if sig:
            s = self.esem[eng]
            s.count += 1
            self.rec[eng].append(("i", fn, s, 1))
            return Tok(s, s.count)
        self.rec[eng].append(("i", fn, None, 0))
        return None

    def dma(self, eng, out, in_, sem, waits=(), **kw):
        self.wait(eng, waits)
        sem.count += 16
        self.rec[eng].append(("i", lambda e, out=out, in_=in_, kw=kw: e.dma_start(out=out, in_=in_, **kw), sem, 16))
        return Tok(sem, sem.count)

    def collective(self, kind, in_ap, out_ap, sem, waits=()):
        self.wait("pool", waits)
        sem.count += 1
        self.rec["pool"].append(("c", lambda e, kind=kind, in_ap=in_ap, out_ap=out_ap: e.collective_compute(
            kind, ALU.bypass, replica_groups=[list(range(NCORES))], ins=[in_ap], outs=[out_ap]), sem, 1))
        return Tok(sem, sem.count)

    def flush(self):
        if any(self.rec[n] for n in self.ENG):
            rec = self.rec

            def replay(items):
                def run(e):
                    for it in items:
                        if it[0] == "w":
                            e.wait_ge(it[1].h, it[2])
                        else:
                            ins = it[1](e)
                            if it[2] is not None:
                                if it[0] == "c":
                                    ins.then_inc(it[2].h)
                                else:
                                    ins.then_inc(it[2].h, it[3])
                return run

            with self.nc.Block() as block:
                if rec["pe"]:
                    block.tensor(replay(rec["pe"]))
                if rec["act"]:
                    block.scalar(replay(rec["act"]))
                if rec["dve"]:
                    block.vector(replay(rec["dve"]))
                if rec["pool"]:
                    block.gpsimd(replay(rec["pool"]))
                if rec["sp"]:
                    block.sync(replay(rec["sp"]))
        self.epoch_es.close()
        self.epoch_es = ExitStack()
        self.epoch += 1
        self.waited = {}
        self.rec = {n: [] for n in self.ENG}


class Ring:
    def __init__(self, bufs):
        self.bufs = bufs
        self.free = [[] for _ in bufs]
        self.i = 0

    def next(self):
        j = self.i % len(self.bufs)
        self.i += 1
        fr = self.free[j]
        self.free[j] = []
        return j, self.bufs[j], fr

    def release(self, j, toks):
        self.free[j] = list(self.free[j]) + [t for t in toks if t is not None]


class WStream:
    def __init__(self, K, nslots, name="w"):
        self.K = K
        self.n = nslots
        self.bufs = [K.sb(f"{name}{i}", [128, 16, CW], BF16) for i in range(nslots)]
        self.sems = [K.sem(f"s_{name}{i}") for i in range(nslots)]
        self.free = [[] for _ in range(nslots)]
        self.released = [True] * nslots
        self.plan_list = []
        self.next_issue = 0
        self.next_pop = 0
        self.issued = {}

    def plan(self, blocks):
        self.plan_list.extend(blocks)

    def _issue_more(self):
        while self.next_issue < len(self.plan_list):
            j = self.next_issue % self.n
            if not self.released[j]:
                break
            src_ap, nk, ncols = self.plan_list[self.next_issue]
            tok = self.K.dma("pool", self.bufs[j][:, 0:nk, 0:ncols], src_ap.rearrange("(k p) c -> p k c", p=128),
                             self.sems[j], waits=self.free[j])
            self.free[j] = []
            self.released[j] = False
            self.issued[self.next_issue] = tok
            self.next_issue += 1

    def pop(self):
        self._issue_more()
        b = self.next_pop
        self.next_pop += 1
        j = b % self.n
        return j, self.bufs[j], self.issued.pop(b)

    def release(self, j, toks):
        self.free[j] = [t for t in toks if t is not None]
        self.released[j] = True
        self._issue_more()


def rstd_from_stat(K, stat_ps, stat_tok, rstd, width, prev_readers=()):
    t1 = K.emit("act", lambda e: e.activation(out=rstd[:, 0:width], in_=stat_ps[:, 0:width], func=AF.Sqrt,
                                               bias=K.eps_t[:, 0:1], scale=1.0 / D),
                waits=[stat_tok] + list(prev_readers))
    t2 = K.emit("dve", lambda e: e.reciprocal(out=rstd[:, 0:width], in_=rstd[:, 0:width]), waits=[t1])
    return t1, t2


def post_plan(w_o, w_in, w_out):
    blocks = []
    for dg in range(D // CW):
        blocks.append((w_o[:, dg * CW:(dg + 1) * CW], 16, CW))
    for jg in range(DFF // CW):
        blocks.append((w_in[:, jg * CW:(jg + 1) * CW], 16, CW))
        blocks.append((w_in[:, DFF + jg * CW:DFF + (jg + 1) * CW], 16, CW))
    for dg in range(D // CW):
        for rb, n in enumerate([2048, 2048, 1536]):
            blocks.append((w_out[rb * 2048:rb * 2048 + n, dg * CW:(dg + 1) * CW], n // 128, CW))
    return blocks


def post_sublayers(K, half, outT, xT_dram, xTout_dram, w_o, w_in, w_out, ws, G1, A2, B2, G2, PS, st):
    nc = K.nc
    T0 = half * 512
    W = 512
    xT = st["xT"]
    ysb = st["ysb"]
    hT = st["hT"]
    aT = st["aT"]
    rstd = st["rstd"]
    tmp_ring = st["tmp_ring"]
    sq_ring = st["sq_ring"]
    sg_ring = st["sg_ring"]
    bankA = st["bankA"]
    stat_y = PS[6]
    stat_x = PS[7]

    x_ready = K.dma("sp", xT[:, :, :], xT_dram[:, T0:T0 + W].rearrange("(k p) t -> p k t", p=128), st["s_x"],
                    waits=st["xT_free"])
    st["xT_free"] = []

    def project_and_stats(nk, rhs_of_k, rhs_tok, wsrc_of_block, nblocks_rows, stat_ps, stat_free):
        y_toks = []
        last_stat = None
        nsq = 0
        for dg in range(D // CW):
            banks = []
            for dcl in range(NDC):
                bj, bank, bfree = bankA.next()
                banks.append((bj, bank, bfree))
            kk = 0
            nrb = len(nblocks_rows)
            for rb in range(nrb):
                nkb = nblocks_rows[rb]
                wj, wbuf, wtok = ws.pop()
                last = []
                for kc in range(nkb):
                    for dcl in range(NDC):
                        bj, bank, bfree = banks[dcl]
                        first = (kk == 0)
                        lastk = (kk == nk - 1)
                        t = K.emit("pe", lambda e, bank=bank, wbuf=wbuf, kc=kc, dcl=dcl, kk=kk, first=first, lastk=lastk:
                                   e.matmul(bank[:, 0:W], lhsT=wbuf[:, kc, dcl * 128:(dcl + 1) * 128], rhs=rhs_of_k(kk),
                                            start=first, stop=lastk),
                                   waits=[wtok, rhs_tok] + (bfree if first else []),
                                   sig=(lastk or kc == nkb - 1))
                        if kc == nkb - 1:
                            last.append(t)
                        if lastk:
                            banks[dcl] = (bj, bank, t)
                    kk += 1
                ws.release(wj, last)
            for dcl in range(NDC):
                dc = dg * NDC + dcl
                bj, bank, mt = banks[dcl]
                t_cp = K.emit("act", lambda e, bank=bank, dc=dc: e.activation(out=ysb[:, dc, :], in_=bank[:, 0:W], func=AF.Copy),
                              waits=[mt] + st["ysb_free"][dc])
                st["ysb_free"][dc] = []
                sj, sq, sfree = sq_ring.next()
                t_sq = K.emit("dve", lambda e, dc=dc, sq=sq: e.tensor_tensor(out=sq[:, :], in0=ysb[:, dc, :], in1=ysb[:, dc, :], op=ALU.mult),
                              waits=[t_cp] + sfree)
                bankA.release(bj, [t_cp])
                t_st = K.emit("pe", lambda e, sq=sq, nsq=nsq: e.matmul(stat_ps[:, 0:W], lhsT=K.ones_f[:, :], rhs=sq[:, :],
                                                                        start=(nsq == 0), stop=(nsq == 15)),
                              waits=[t_sq] + (stat_free if nsq == 0 else []))
                sq_ring.release(sj, [t_st])
                nsq += 1
                last_stat = t_st
                y_toks.append(t_cp)
        return y_toks, last_stat

    def residual_update(y_toks, stat_ps, stat_tok, G):
        t1, t_r = rstd_from_stat(K, stat_ps, stat_tok, rstd, W, prev_readers=st["rstd_readers"])
        st["rstd_readers"] = []
        x_toks = []
        for k in range(KD):
            tj, tmp, tfree = tmp_ring.next()
            t_a = K.emit("dve", lambda e, k=k, tmp=tmp: e.scalar_tensor_tensor(out=tmp[:, :], in0=ysb[:, k, :], scalar=G[:, k:k + 1],
                                                                                 in1=rstd[:, 0:W], op0=ALU.mult, op1=ALU.mult),
                         waits=[t_r, y_toks[k]] + tfree)
            t_b = K.emit("pool", lambda e, k=k, tmp=tmp: e.tensor_tensor(out=xT[:, k, :], in0=xT[:, k, :], in1=tmp[:, :], op=ALU.add),
                         waits=[t_a, x_ready] + st["xk_readers"][k])
            st["xk_readers"][k] = []
            tmp_ring.release(tj, [t_b])
            st["ysb_free"][k] = [t_a]
            st["rstd_readers"].append(t_a)
            x_toks.append(t_b)
        return x_toks, t1

    yt, stt = project_and_stats(KD, lambda kk: outT[:, kk, T0:T0 + W], st["outT_tok"],
                                lambda rb, dg: w_o[:, dg * CW:(dg + 1) * CW], [16], stat_y, st["stat_y_free"])
    x_toks, t_sr = residual_update(yt, stat_y, stt, G1)
    st["stat_y_free"] = [t_sr]

    last = None
    for k in range(KD):
        sj, sq, sfree = sq_ring.next()
        t_sq = K.emit("act", lambda e, k=k, sq=sq: e.activation(out=sq[:, :], in_=xT[:, k, :], func=AF.Square),
                      waits=[x_toks[k]] + sfree)
        t_st = K.emit("pe", lambda e, sq=sq, k=k: e.matmul(stat_x[:, 0:W], lhsT=K.ones_f[:, :], rhs=sq[:, :], start=(k == 0), stop=(k == KD - 1)),
                      waits=[t_sq] + (st["stat_x_free"] if k == 0 else []))
        sq_ring.release(sj, [t_st])
        st["xk_readers"][k].append(t_sq)
        last = t_st
    t1, t_r = rstd_from_stat(K, stat_x, last, rstd, W, prev_readers=st["rstd_readers"])
    st["rstd_readers"] = []
    st["stat_x_free"] = [t1]
    h_toks = []
    for k in range(KD):
        tj, tmp, tfree = tmp_ring.next()
        t_a = K.emit("dve", lambda e, k=k, tmp=tmp: e.scalar_tensor_tensor(out=tmp[:, :], in0=xT[:, k, :], scalar=A2[:, k:k + 1],
                                                                             in1=rstd[:, 0:W], op0=ALU.mult, op1=ALU.mult),
                     waits=[t_r, x_toks[k]] + tfree)
        t_b = K.emit("act", lambda e, k=k, tmp=tmp: e.activation(out=hT[:, k, :], in_=tmp[:, :], func=AF.Identity, bias=B2[:, k:k + 1], scale=1.0),
                     waits=[t_a] + st["hT_readers"])
        tmp_ring.release(tj, [t_b])
        st["xk_readers"][k].append(t_a)
        st["rstd_readers"].append(t_a)
        h_toks.append(t_b)
    st["hT_readers"] = []
    h_all = h_toks

    a_toks = [None] * NF
    for jg in range(DFF // CW):
        uj, ubuf, utok = ws.pop()
        gj, gbuf, gtok = ws.pop()
        ulast = []
        glast = []
        for jl in range(NDC):
            j = jg * NDC + jl
            ubj, ubank, ufree = bankA.next()
            gbj, gbank, gfree = bankA.next()
            for k in range(KD):
                tu = K.emit("pe", lambda e, k=k, ubank=ubank, ubuf=ubuf, jl=jl: e.matmul(ubank[:, 0:W], lhsT=ubuf[:, k, jl * 128:(jl + 1) * 128], rhs=hT[:, k, :],
                                                                                         start=(k == 0), stop=(k == KD - 1)),
                            waits=[utok] + h_all + (ufree if k == 0 else []), sig=(k == KD - 1))
            for k in range(KD):
                tg = K.emit("pe", lambda e, k=k, gbank=gbank, gbuf=gbuf, jl=jl: e.matmul(gbank[:, 0:W], lhsT=gbuf[:, k, jl * 128:(jl + 1) * 128], rhs=hT[:, k, :],
                                                                                         start=(k == 0), stop=(k == KD - 1)),
                            waits=[gtok] + (gfree if k == 0 else []), sig=(k == KD - 1))
            ulast.append(tu)
            glast.append(tg)
            sj, sg, sfree = sg_ring.next()
            t_s = K.emit("act", lambda e, gbank=gbank, sg=sg: e.activation(out=sg[:, :], in_=gbank[:, 0:W], func=AF.Silu),
                         waits=[tg] + sfree)
            t_m = K.emit("dve", lambda e, ubank=ubank, sg=sg, j=j: e.tensor_tensor(out=aT[:, j, :], in0=ubank[:, 0:W], in1=sg[:, :], op=ALU.mult),
                         waits=[tu, t_s] + st["aT_readers"][j])
            st["aT_readers"][j] = []
            sg_ring.release(sj, [t_m])
            bankA.release(ubj, [t_m])
            bankA.release(gbj, [t_s])
            a_toks[j] = t_m
        ws.release(uj, ulast)
        ws.release(gj, glast)
    st["hT_readers"] = list(ulast) + list(glast)

    yt, stt = project_and_stats(NF, lambda kk: aT[:, kk, :], a_toks,
                                lambda rb, dg: w_out[rb * 2048:rb * 2048 + [2048, 2048, 1536][rb], dg * CW:(dg + 1) * CW],
                                [16, 16, 12], stat_y, st["stat_y_free"])
    for j in range(NF):
        st["aT_readers"][j] = [stt]
    x_toks, t_sr = residual_update(yt, stat_y, stt, G2)
    st["stat_y_free"] = [t_sr]
    t_o = K.dma("sp", xTout_dram[:, T0:T0 + W].rearrange("(k p) t -> p k t", p=128), xT[:, :, :], st["s_xo"], waits=x_toks)
    st["xT_free"] = [t_o]
    return t_o


def setup_common(K):
    nc = K.nc
    K.ones_f = K.sb("ones_f", [128, 128], F32)
    K.eps_t = K.sb("eps_t", [128, 1], F32)
    K.PS = [K.es.enter_context(nc.psum_tensor(f"{K.pfx}ps{i}", [128, 512], F32)) for i in range(8)]
    t1 = K.emit("dve", lambda e: e.memset(K.ones_f[:, :], 1.0))
    t2 = K.emit("dve", lambda e: e.memset(K.eps_t[:, :], EPS))
    K.const_tok = [t1, t2]
    for eng in ("pe", "act", "dve", "pool"):
        K.wait(eng, K.const_tok)


def make_post_state(K, nslots=4):
    st = {}
    st["xT"] = K.sb("xT_sb", [128, KD, 512], F32)
    st["ysb"] = K.sb("ysb", [128, KD, 512], F32)
    st["hT"] = K.sb("hT", [128, KD, 512], BF16)
    st["aT"] = K.sb("aT", [128, NF, 512], BF16)
    st["rstd"] = K.sb("rstd", [128, 512], F32)
    st["tmp_ring"] = Ring([K.sb(f"tmp{i}", [128, 512], F32) for i in range(2)])
    st["sq_ring"] = Ring([K.sb(f"sq{i}", [128, 512], F32) for i in range(2)])
    st["sg_ring"] = Ring([K.sb(f"sg{i}", [128, 512], F32) for i in range(2)])
    st["bankA"] = Ring(K.PS[0:6])
    st["s_x"] = K.sem("s_x")
    st["s_xo"] = K.sem("s_xo")
    st["xT_free"] = []
    st["ysb_free"] = [[] for _ in range(KD)]
    st["xk_readers"] = [[] for _ in range(KD)]
    st["rstd_readers"] = []
    st["hT_readers"] = []
    st["aT_readers"] = [[] for _ in range(NF)]
    st["stat_y_free"] = []
    st["stat_x_free"] = []
    st["ws"] = WStream(K, nslots)
    return st


def build_mod():
    nc = bass.Bass("TRN2", target_bir_lowering=False)
    T = {"cT": nc.dram_tensor("cT", [128, KD], F32, kind="ExternalInput").ap(),
         "adaw": nc.dram_tensor("adaw", [D, 6144], F32, kind="ExternalInput").ap(),
         "adab": nc.dram_tensor("adab", [128, 48], F32, kind="ExternalInput").ap(),
         "modo": nc.dram_tensor("modo", [128, 48], F32, kind="ExternalOutput").ap()}
    emit_mod(nc, T, "")
    return nc


def emit_mod(nc, T, pfx):
    cT_d, w_d, b_d, o_d = T["cT"], T["adaw"], T["adab"], T["modo"]
    with ExitStack() as es:
        K = Ctx(nc, es, pfx)
        ps = es.enter_context(nc.psum_tensor(K.pfx + "psm", [128, 512], F32))
        cT = K.sb("cT_sb", [128, KD], F32)
        bsb = K.sb("b_sb", [128, 48], F32)
        osb = K.sb("o_sb", [128, 48], F32)
        wb = [K.sb(f"wm{i}", [128, KD, 512], F32) for i in range(3)]
        wsem = [K.sem(f"s_wm{i}") for i in range(3)]
        s_in = K.sem("s_in")
        s_out = K.sem("s_out")
        t_c = K.dma("sp", cT[:, :], cT_d, s_in)
        t_b = K.dma("sp", bsb[:, :], b_d, s_in)
        t_act = K.emit("act", lambda e: e.activation(out=cT[:, :], in_=cT[:, :], func=AF.Silu), waits=[t_b])
        ring = Ring(wb)
        last = None
        for blk in range(12):
            j, buf, fr = ring.next()
            eng = "sp" if blk % 2 == 0 else "act"
            tw = K.dma(eng, buf[:, :, :], w_d[:, blk * 512:(blk + 1) * 512].rearrange("(k p) c -> p k c", p=128), wsem[j], waits=fr)
            for cl in range(4):
                col = blk * 4 + cl
                for k in range(KD):
                    t = K.emit("pe", lambda e, buf=buf, cl=cl, k=k, col=col: e.matmul(ps[:, col:col + 1], lhsT=buf[:, k, cl * 128:(cl + 1) * 128],
                                                                                      rhs=cT[:, k:k + 1], start=(k == 0), stop=(k == KD - 1)),
                               waits=[tw, t_act], sig=(k == KD - 1 and cl == 3))
            ring.release(j, [t])
            last = t
        t_o = K.emit("dve", lambda e: e.tensor_tensor(out=osb[:, :], in0=ps[:, 0:48], in1=bsb[:, :], op=ALU.add), waits=[last, t_b])
        t_d = K.dma("sp", o_d, osb[:, :], s_out, waits=[t_o])
        K.wait("sp", [t_d])
        K.flush()


TWO_PI = 2.0 * np.pi


def rope_tables(K, pos_rep_d, fvec, name, scr, after=()):
    C = K.sb(name + "_C", [128, TL], F32)
    Sn = K.sb(name + "_S", [128, TL], F32)
    posi, ang, u, ui = scr["posi"], scr["ang"], scr["u"], scr["ui"]
    tp = K.dma("sp", posi[:, :], pos_rep_d, scr["sem"], waits=list(after))
    t0 = K.emit("dve", lambda e: e.tensor_copy(out=ang[:, :], in_=posi[:, :]), waits=[tp] + list(after))
    t1 = K.emit("dve", lambda e: e.tensor_scalar(out=ang[:, :], in0=ang[:, :], scalar1=fvec, scalar2=None, op0=ALU.mult), waits=[t0])
    last = t1
    toks = []
    for which, dst in ((0, Sn), (1, C)):
        off = 0.0 if which == 0 else np.pi / 2
        ta = K.emit("dve", lambda e, off=off: e.tensor_scalar(out=u[:, :], in0=ang[:, :], scalar1=float(off), scalar2=float(1.0 / TWO_PI),
                                                                op0=ALU.add, op1=ALU.mult), waits=[last])
        tb = K.emit("dve", lambda e: e.tensor_copy(out=ui[:, :], in_=u[:, :]), waits=[ta])
        tc = K.emit("dve", lambda e: e.tensor_copy(out=u[:, :], in_=ui[:, :]), waits=[tb])
        td = K.emit("dve", lambda e: e.scalar_tensor_tensor(out=u[:, :], in0=u[:, :], scalar=float(-TWO_PI), in1=ang[:, :],
                                                             op0=ALU.mult, op1=ALU.add), waits=[tc])
        te = K.emit("dve", lambda e, off=off, dst=dst: e.tensor_scalar(out=dst[:, :], in0=u[:, :], scalar1=float(off), scalar2=None, op0=ALU.add),
                    waits=[td])
        tf = K.emit("dve", lambda e, dst=dst: e.tensor_scalar(out=u[:, :], in0=dst[:, :], scalar1=float(np.pi), scalar2=float(-TWO_PI),
                                                                op0=ALU.is_ge, op1=ALU.mult), waits=[te])
        tg = K.emit("dve", lambda e, dst=dst: e.tensor_tensor(out=dst[:, :], in0=dst[:, :], in1=u[:, :], op=ALU.add), waits=[tf])
        th = K.emit("dve", lambda e, dst=dst: e.tensor_scalar(out=dst[:, :], in0=dst[:, :], scalar1=float(-3.14159), scalar2=float(3.14159),
                                                                op0=ALU.max, op1=ALU.min), waits=[tg])
        ti = K.emit("act", lambda e, dst=dst: e.activation(out=dst[:, :], in_=dst[:, :], func=AF.Sin), waits=[th])
        last = th
        toks.append(ti)
    return C, Sn, toks, last


def norm_modulate(K, xT, x_tok, A, Bv, hT, TW, sq_ring, tmp_ring, rstd, stat_banks):
    ng = TW // 512
    last = [None] * ng
    for k in range(KD):
        for g in range(ng):
            sj, sq, sfree = sq_ring.next()
            t_sq = K.emit("act", lambda e, k=k, sq=sq, g=g: e.activation(out=sq[:, :], in_=xT[:, k, g * 512:(g + 1) * 512], func=AF.Square),
                          waits=[x_tok] + sfree)
            t_st = K.emit("pe", lambda e, sq=sq, k=k, g=g: e.matmul(stat_banks[g][:, 0:512], lhsT=K.ones_f[:, :], rhs=sq[:, :],
                                                                    start=(k == 0), stop=(k == KD - 1)), waits=[t_sq])
            sq_ring.release(sj, [t_st])
            last[g] = t_st
    t_r = []
    for g in range(ng):
        t1 = K.emit("act", lambda e, g=g: e.activation(out=rstd[:, g * 512:(g + 1) * 512], in_=stat_banks[g][:, 0:512], func=AF.Sqrt,
                                                        bias=K.eps_t[:, 0:1], scale=1.0 / D), waits=[last[g]])
        t2 = K.emit("dve", lambda e, g=g: e.reciprocal(out=rstd[:, g * 512:(g + 1) * 512], in_=rstd[:, g * 512:(g + 1) * 512]), waits=[t1])
        t_r.append(t2)
    h_toks = []
    for k in range(KD):
        for g in range(ng):
            tj, tmp, tfree = tmp_ring.next()
            t_a = K.emit("dve", lambda e, k=k, tmp=tmp, g=g: e.scalar_tensor_tensor(out=tmp[:, :], in0=xT[:, k, g * 512:(g + 1) * 512], scalar=A[:, k:k + 1],
                                                                                      in1=rstd[:, g * 512:(g + 1) * 512], op0=ALU.mult, op1=ALU.mult),
                         waits=[t_r[g], x_tok] + tfree)
            t_b = K.emit("act", lambda e, k=k, tmp=tmp, g=g: e.activation(out=hT[:, k, g * 512:(g + 1) * 512], in_=tmp[:, :], func=AF.Identity,
                                                                           bias=Bv[:, k:k + 1], scale=1.0), waits=[t_a])
            tmp_ring.release(tj, [t_b])
            h_toks.append(t_b)
    return h_toks[-1]


def pre_dims(kind):
    if kind == "A":
        return 2048 + 512 + 1024 + 64 + 512 + 16, 512
    return 6144, 2048


def build_pre(kind, stage=9):
    nc = bass.Bass("TRN2", target_bir_lowering=False)
    ncols, n_tm = pre_dims(kind)
    T = {}
    T["xT"] = nc.dram_tensor("xT", [D, TL], F32, kind="ExternalInput").ap()
    vecs_d = nc.dram_tensor("vecs", [128, 50], F32, kind="ExternalInput").ap()
    T["vec_loader"] = lambda K, vecs, sem: K.dma("sp", vecs[:, :], vecs_d, sem)
    T["posrep"] = nc.dram_tensor("posrep", [128, TL], I32, kind="ExternalInput").ap()
    T["w"] = nc.dram_tensor("w", [D, ncols], F32, kind="ExternalInput").ap()
    T["rconst"] = nc.dram_tensor("rconst", [128, 256], BF16, kind="ExternalInput").ap()
    T["q"] = nc.dram_tensor("qT", [2048, TL], BF16, kind="ExternalOutput").ap()
    if kind == "A":
        T["k"] = nc.dram_tensor("kT", [512, TL], BF16, kind="ExternalOutput").ap()
        T["qi"] = nc.dram_tensor("qiT", [1024, TL], BF16, kind="ExternalOutput").ap()
        T["ki"] = nc.dram_tensor("kiT", [64, TL], BF16, kind="ExternalOutput").ap()
        T["v"] = nc.dram_tensor("v", [TL, 512], BF16, kind="ExternalOutput").ap()
        T["wi"] = nc.dram_tensor("wi", [TL, 16], F32, kind="ExternalOutput").ap()
    else:
        T["k"] = nc.dram_tensor("kT", [2048, TL], BF16, kind="ExternalOutput").ap()
        T["v"] = nc.dram_tensor("v", [TL, 2048], BF16, kind="ExternalOutput").ap()
        T["ksum"] = nc.dram_tensor("ksum", [128, 16 * 32], F32, kind="ExternalOutput").ap()
    emit_pre(nc, kind, T, "", stage)
    return nc


def emit_pre(nc, kind, T, pfx, stage=9):
    if kind == "A":
        fm = [("q", i, 128, 0) for i in range(16)] + [("k", i, 128, 0) for i in range(4)] + [("qi", i, 128, 1) for i in range(8)] + [("ki", 0, 64, 1)]
    else:
        fm = [("q", i, 128, 0) for i in range(16)] + [("k", i, 128, 0) for i in range(16)]
    ncols, n_tm = pre_dims(kind)
    xT_d, pos_d, w_d, rc_d = T["xT"], T["posrep"], T["w"], T["rconst"]
    outs = T
    v_d = T["v"]
    wi_d = T.get("wi")
    ks_d = T.get("ksum")
    with ExitStack() as es:
        K = Ctx(nc, es, pfx)
        setup_common(K)
        PS = K.PS
        xT = K.sb("xT_sb", [128, KD, TL], F32)
        hT = K.sb("hT", [128, KD, TL], BF16)
        vecs = K.sb("vecs_sb", [128, 50], F32)
        rc = K.sb("rc_sb", [128, 256], BF16)
        rstd = K.sb("rstd", [128, TL], F32)
        sq_ring = Ring([K.sb(f"sq{i}", [128, 512], F32) for i in range(2)])
        tmp_ring = Ring([K.sb(f"tmp{i}", [128, 512], F32) for i in range(2)])
        qraw_ring = Ring([K.sb(f"qraw{i}", [128, 512], BF16) for i in range(3)])
        t2_ring = Ring([K.sb(f"t2_{i}", [128, 512], F32) for i in range(3)])
        qout_ring = Ring([K.sb(f"qout{i}", [128, TL], BF16) for i in range(3)])
        vst_ring = Ring([K.sb(f"vst{i}", [128, 256], BF16) for i in range(3)])
        scr = {"posi": K.sb("posi", [128, TL], I32), "ang": K.sb("ang", [128, TL], F32), "u": K.sb("u_s", [128, TL], F32),
               "ui": K.sb("ui", [128, TL], I32), "sem": K.sem("s_pos")}
        ws = WStream(K, 4)
        s_in = K.sem("s_in")
        s_out = K.sem("s_out")
        s_qo = [K.sem(f"s_qo{i}") for i in range(3)]
        s_vo = [K.sem(f"s_vo{i}") for i in range(3)]
        out_toks = []
        K.dma("sp", xT[:, :, :], xT_d.rearrange("(k p) t -> p k t", p=128), s_in)
        T["vec_loader"](K, vecs, s_in)
        K.dma("sp", rc[:, :], rc_d, s_in)
        x_tok = Tok(s_in, s_in.count)
        for eng in ("dve", "act", "pe", "pool"):
            K.wait(eng, [x_tok])
        t_A = K.emit("dve", lambda e: e.scalar_tensor_tensor(out=vecs[:, 0:16], in0=vecs[:, 0:16], scalar=1.0, in1=vecs[:, 32:48], op0=ALU.add, op1=ALU.mult),
                     waits=[x_tok])
        K.wait("act", [t_A])
        h_tok = norm_modulate(K, xT, x_tok, vecs[:, 0:16], vecs[:, 16:32], hT, TL, sq_ring, tmp_ring, rstd, [PS[6], PS[7]])
        Cm, Sm, tk_m, lastm = rope_tables(K, pos_d, vecs[:, 48:49], "rm", scr)
        tabs = [(Cm, Sm, tk_m)]
        if kind == "A":
            Ci, Si, tk_i, lasti = rope_tables(K, pos_d, vecs[:, 49:50], "ri", scr, after=[lastm])
            tabs.append((Ci, Si, tk_i))
        if kind == "B":
            ksum = K.sb("ksum_sb", [128, 16, 32], F32)
        if stage <= 2:
            fm = []
        acc_ring = Ring(PS[0:4])
        rq_ring = Ring(PS[4:6])
        pl = []
        c_ = 0
        i_ = 0
        while i_ < len(fm):
            bw_ = sum(c[2] for c in fm[i_:i_ + NDC])
            pl.append((w_d[:, c_:c_ + bw_], KD, bw_))
            c_ += bw_
            i_ += NDC
        for c0 in range(0, n_tm if stage > 3 else 0, 256):
            pl.append((w_d[:, c_ + c0:c_ + c0 + 256], KD, 256))
        if kind == "A" and stage > 3:
            pl.append((w_d[:, c_ + n_tm:c_ + n_tm + 16], KD, 16))
        ws.plan(pl)
        col = 0
        ci = 0
        ks_toks = []
        while ci < len(fm):
            blk = fm[ci:ci + NDC]
            bw = sum(c[2] for c in blk)
            wj, wbuf, wtok = ws.pop()
            wl = []
            off = 0
            for (nm, idx, M, rk) in blk:
                Ct, St, tk = tabs[rk]
                R = rc[:, rk * 128:(rk + 1) * 128]
                banks = [acc_ring.next() for _ in range(2)]
                mt = [None, None]
                for k in range(KD):
                    for g in range(2):
                        bj, bank, bfree = banks[g]
                        mt[g] = K.emit("pe", lambda e, bank=bank, k=k, g=g, off=off, M=M, wbuf=wbuf: e.matmul(
                            bank[0:M, 0:512], lhsT=wbuf[:, k, off:off + M], rhs=hT[:, k, g * 512:(g + 1) * 512], start=(k == 0), stop=(k == KD - 1)),
                            waits=[wtok, h_tok] + (bfree if k == 0 else []), sig=(k == KD - 1))
                wl.append(mt[1])
                oj, qout, ofree = qout_ring.next()
                fin = []
                for g in range(2):
                    bj, bank, _ = banks[g]
                    rj, qraw, rfree = qraw_ring.next()
                    t_raw = K.emit("act", lambda e, bank=bank, qraw=qraw, M=M: e.activation(out=qraw[0:M, :], in_=bank[0:M, 0:512], func=AF.Copy),
                                   waits=[mt[g]] + rfree)
                    tj, t2, t2free = t2_ring.next()
                    t_t2 = K.emit("dve", lambda e, bank=bank, t2=t2, M=M, g=g, Ct=Ct: e.tensor_tensor(out=t2[0:M, :], in0=bank[0:M, 0:512],
                                                                                                      in1=Ct[0:M, g * 512:(g + 1) * 512], op=ALU.mult),
                                 waits=[mt[g], tk, t_raw] + t2free)
                    acc_ring.release(bj, [t_raw, t_t2])
                    qj, rq, rqfree = rq_ring.next()
                    t_rq = K.emit("pe", lambda e, rq=rq, qraw=qraw, M=M, R=R: e.matmul(rq[0:M, 0:512], lhsT=R[0:M, 0:M], rhs=qraw[0:M, :], start=True, stop=True),
                                  waits=[t_raw] + rqfree)
                    qraw_ring.release(rj, [t_rq])
                    mj, tmp, tfree = tmp_ring.next()
                    t_m = K.emit("dve", lambda e, rq=rq, tmp=tmp, M=M, g=g, St=St: e.tensor_tensor(out=tmp[0:M, :], in0=rq[0:M, 0:512],
                                                                                                   in1=St[0:M, g * 512:(g + 1) * 512], op=ALU.mult),
                                waits=[t_rq, tk] + tfree)
                    rq_ring.release(qj, [t_m])
                    t_f = K.emit("pool", lambda e, tmp=tmp, t2=t2, qout=qout, M=M, g=g: e.tensor_tensor(out=qout[0:M, g * 512:(g + 1) * 512], in0=tmp[0:M, :],
                                                                                                       in1=t2[0:M, :], op=ALU.add),
                                waits=[t_m, t_t2] + (ofree if g == 0 else []))
                    tmp_ring.release(mj, [t_f])
                    t2_ring.release(tj, [t_f])
                    fin.append(t_f)
                rel = []
                if kind == "B" and nm == "k":
                    t_ks = K.emit("dve", lambda e, qout=qout, idx=idx: e.tensor_reduce(out=ksum[:, idx, :], in_=qout[:, :].rearrange("p (b s) -> p b s", s=32),
                                                                                       axis=AX.X, op=ALU.add), waits=fin)
                    ks_toks.append(t_ks)
                    rel.append(t_ks)
                t_o = K.dma("sp", outs[nm][idx * 128:idx * 128 + M, :], qout[0:M, :], s_qo[oj], waits=fin)
                rel.append(t_o)
                out_toks.append(t_o)
                qout_ring.release(oj, rel)
                off += M
            ws.release(wj, wl)
            col += bw
            ci += len(blk)
        tm_blocks = [(c0, 256, "v") for c0 in range(0, n_tm, 256)]
        if stage <= 3:
            tm_blocks = []
        if kind == "A" and stage > 3:
            tm_blocks.append((n_tm, 16, "wi"))
            wist = K.sb("wist", [128, 8, 16], F32)
        for (c0, bw, nm) in tm_blocks:
            wj, wbuf, wtok = ws.pop()
            wl = []
            for tt in range(8):
                bj, bank, bfree = acc_ring.next()
                for k in range(KD):
                    t = K.emit("pe", lambda e, bank=bank, k=k, tt=tt, bw=bw, wbuf=wbuf: e.matmul(bank[:, 0:bw], lhsT=hT[:, k, tt * 128:(tt + 1) * 128],
                                                                                              rhs=wbuf[:, k, 0:bw], start=(k == 0), stop=(k == KD - 1)),
                               waits=[wtok, h_tok] + (bfree if k == 0 else []), sig=(k == KD - 1))
                wl.append(t)
                if nm == "v":
                    vj, vst, vfree = vst_ring.next()
                    t_c = K.emit("act", lambda e, bank=bank, vst=vst, bw=bw: e.activation(out=vst[:, 0:bw], in_=bank[:, 0:bw], func=AF.Copy), waits=[t] + vfree)
                    t_o = K.dma("sp", v_d[tt * 128:(tt + 1) * 128, c0:c0 + bw], vst[:, 0:bw], s_vo[vj], waits=[t_c])
                    vst_ring.release(vj, [t_o])
                    out_toks.append(t_o)
                else:
                    t_c = K.emit("act", lambda e, bank=bank, tt=tt: e.activation(out=wist[:, tt, :], in_=bank[:, 0:16], func=AF.Copy), waits=[t])
                    if tt == 7:
                        t_o = K.dma("sp", wi_d.rearrange("(a p) c -> p a c", p=128), wist[:, :, :], s_out, waits=[t_c])
                        out_toks.append(t_o)
                acc_ring.release(bj, [t_c])
            ws.release(wj, wl)
        if stage <= 2:
            K.wait("sp", tk_m)
        if kind == "B" and stage > 2:
            t_o = K.dma("sp", ks_d, ksum[:, :, :].rearrange("p a b -> p (a b)"), s_out, waits=ks_toks)
            out_toks.append(t_o)
        K.wait("sp", [Tok(s_out, s_out.count)] + [Tok(x, x.count) for x in s_qo + s_vo])
        K.flush()


ROPE_THETA = 500000.0


def rope_consts():
    f = np.zeros((128, 2), np.float32)
    R = np.zeros((128, 256), np.float32)
    for p in range(32):
        f[p, 0] = ROPE_THETA ** (-(p % 16) / 16.0)
    for d in range(16):
        R[d + 16, d] = -1.0
        R[d, d + 16] = 1.0
    for blk in range(2):
        b0 = blk * 64
        for p in range(16):
            f[b0 + p, 1] = ROPE_THETA ** (-(p % 8) / 8.0)
        for d in range(8):
            R[b0 + d + 8, 128 + b0 + d] = -1.0
            R[b0 + d, 128 + b0 + d + 8] = 1.0
    return f, R.astype(ml_dtypes.bfloat16)


def vec_layout(v):
    return np.ascontiguousarray(np.asarray(v).reshape(KD, 128).T)


NEG = -60000.0
SCALE = 128 ** -0.5


def barrier(K):
    toks = [Tok(s, s.count) for s in K.esem.values() if s.count > 0]
    for eng in ("pe", "act", "dve", "pool", "sp"):
        K.wait(eng, toks)


def attention_core(K, heads, tgs, get_kv, qT_of_head, bias_mm, outT, cst, st_banks, acc_pairs, LA=2):
    st_ring = Ring(st_banks)
    acc_ring = Ring(acc_pairs)
    pT_ring = cst["pT_ring"]
    rden_ring = cst["rden_ring"]
    for h in heads:
        kT, vt, kv_tok, kv_release = get_kv(h)
        qf, q_tok = qT_of_head(h)
        work = []
        for tg in tgs:
            qt0 = 4 * tg
            nm = qt0 + 4
            for m in range(nm):
                c0 = 128 * max(0, m - qt0)
                for r in range(8):
                    work.append({"tg": tg, "m": m, "r": r, "c0": c0, "N": 512 - c0, "first": (m == 0 and r == 0), "last": (m == nm - 1 and r == 7)})
        state = {"acc": None, "last_pe": None}

        def stage_a(w):
            tg, m, r, c0, N = w["tg"], w["m"], w["r"], w["c0"], w["N"]
            sj, st, sfree = st_ring.next()
            extra = bias_mm(h, tg, m, r, c0)
            K.emit("pe", lambda e: e.matmul(st[:, c0:512], lhsT=kT[:, r, m * 128:(m + 1) * 128], rhs=qf(tg * 512 + c0, tg * 512 + 512), start=True, stop=False),
                   waits=[kv_tok, q_tok] + sfree, sig=False)
            t_s = None
            for xi, (fn, xw) in enumerate(extra):
                t_s = K.emit("pe", lambda e, fn=fn: fn(e, st), waits=xw, sig=(xi == len(extra) - 1))
            pj, pT, pfree = pT_ring.next()
            t_e = K.emit("act", lambda e: e.activation(out=pT[:, 0:N], in_=st[:, c0:512], func=AF.Exp, scale=SCALE), waits=[t_s] + pfree)
            st_ring.release(sj, [t_e])
            w["pj"], w["pT"], w["t_e"] = pj, pT, t_e

        def stage_b(w):
            tg, m, r, c0, N = w["tg"], w["m"], w["r"], w["c0"], w["N"]
            if w["first"]:
                state["acc"] = acc_ring.next()
            aj, (oacc, dacc), afree = state["acc"]
            pT = w["pT"]
            K.emit("pe", lambda e: e.matmul(oacc[:, c0:512], lhsT=vt[:, r * 8 + m, :], rhs=pT[:, 0:N], start=w["first"], stop=w["last"]),
                   waits=[w["t_e"]] + (afree if w["first"] else []), sig=False)
            t_p = K.emit("pe", lambda e: e.matmul(dacc[:, c0:512], lhsT=cst["ones_bf"][:, :], rhs=pT[:, 0:N], start=w["first"], stop=w["last"]), sig=True)
            pT_ring.release(w["pj"], [t_p])
            state["last_pe"] = t_p
            if w["last"]:
                rj, rden, rfree = rden_ring.next()
                t_r = K.emit("dve", lambda e: e.reciprocal(out=rden[:, :], in_=dacc[:, 0:512]), waits=[t_p] + rfree)
                t_o = K.emit("dve", lambda e: e.tensor_tensor(out=outT[:, h, tg * 512:(tg + 1) * 512], in0=oacc[:, 0:512], in1=rden[:, :], op=ALU.mult), waits=[t_r])
                rden_ring.release(rj, [t_o])
                acc_ring.release(aj, [t_o])
                cst["out_tok"] = t_o

        for n, w in enumerate(work):
            stage_a(w)
            if n >= LA:
                stage_b(work[n - LA])
        for w in work[max(0, len(work) - LA):]:
            stage_b(w)
        kv_release(h, [state["last_pe"]])


def att_consts(core):
    cb = np.zeros((128, 128 + 128 + 1024 + 1024 + 1024), np.float32)
    cb[:, 0:128] = np.eye(128)
    cb[:, 128:256] = 1.0
    for m in range(8):
        for p in range(128):
            cb[4 * m + p // 32, 256 + m * 128 + p] = 1.0
    p = np.arange(128)[:, None]
    t = np.arange(128)[None, :]
    for r in range(8):
        ok = (p < t) | ((p == t) & (r <= core))
        same = (p // 32) == (t // 32)
        cb[:, 1280 + r * 128:1280 + (r + 1) * 128] = np.where(same & ~ok, NEG, 0.0)
        cb[:, 2304 + r * 128:2304 + (r + 1) * 128] = np.where(ok.T, 0.0, -1e30)
    cf = np.zeros((128, 128 + 256 + 256 + 256), np.float32)
    cf[:, 0:128] = np.eye(128)
    for qt in range(8):
        j = 4 * qt + np.arange(128) // 32
        b = np.arange(32)[None, :]
        pv = (b < j[:, None]).astype(np.float32)
        cf[:, 128 + qt * 32:128 + (qt + 1) * 32] = pv
        cf[:, 384 + qt * 32:384 + (qt + 1) * 32] = (b == j[:, None]).astype(np.float32)
        cf[:, 640 + qt * 32:640 + (qt + 1) * 32] = (pv - 1.0) * 1e30
    return cb.astype(ml_dtypes.bfloat16), cf


def build_att(kind, with_post=True):
    nc = bass.Bass("TRN2", target_bir_lowering=False)
    nkv = 4 if kind == "A" else 16
    T = {}
    T["qT"] = nc.dram_tensor("qT", [D, TL], BF16, kind="ExternalInput").ap()
    kT_d = nc.dram_tensor("kTall", [nkv, 128, 8, TL], BF16, kind="ExternalInput").ap()
    v_d = nc.dram_tensor("vall", [nkv, 128, 64, 128], BF16, kind="ExternalInput").ap()

    def kv_loader(K, g, kdst, vdst, sem, waits):
        K.dma("sp", kdst[:, :, :], kT_d[g], sem, waits=waits)
        return K.dma("sp", vdst[:, :, :], v_d[g], sem, waits=waits)
    T["kv_loader"] = kv_loader
    T["cb"] = nc.dram_tensor("cb", [128, 3328], BF16, kind="ExternalInput").ap()
    T["cf"] = nc.dram_tensor("cf", [128, 896], F32, kind="ExternalInput").ap()
    vecs_d = nc.dram_tensor("vecs", [128, 112], F32, kind="ExternalInput").ap()
    T["vec_loader"] = lambda K, vecs, sem: K.dma("sp", vecs[:, :], vecs_d, sem)
    T["xT"] = nc.dram_tensor("xT", [D, TL], F32, kind="ExternalInput").ap()
    T["w_o"] = nc.dram_tensor("w_o", [D, D], F32, kind="ExternalInput").ap()
    T["w_in"] = nc.dram_tensor("w_in", [D, 2 * DFF], F32, kind="ExternalInput").ap()
    T["w_out"] = nc.dram_tensor("w_out", [DFF, D], F32, kind="ExternalInput").ap()
    if kind == "B":
        ks_d = nc.dram_tensor("ksall", [128, 8, 512], F32, kind="ExternalInput").ap()
        T["ks_loader"] = lambda K, ksa, sem: K.dma("sp", ksa[:, :, :], ks_d, sem)
    else:
        T["qi"] = nc.dram_tensor("qiT", [1024, TL], BF16, kind="ExternalInput").ap()
        ki_d = nc.dram_tensor("kiall", [64, 8, 8, 128], BF16, kind="ExternalInput").ap()

        def ki_loader(K, kiT, sem):
            K.dma("sp", kiT[0:64, :, :, :], ki_d, sem)
            K.dma("sp", kiT[64:128, :, :, :], ki_d, sem)
        T["ki_loader"] = ki_loader
        T["wi"] = nc.dram_tensor("wi", [TL, 16], F32, kind="ExternalInput").ap()
    T["xo"] = nc.dram_tensor("xo", [D, TL], F32, kind="ExternalOutput").ap()
    if not with_post:
        T["ao"] = nc.dram_tensor("ao", [D, TL], BF16, kind="ExternalOutput").ap()
    emit_att(nc, kind, T, "", with_post)
    return nc


def emit_att(nc, kind, T, pfx, with_post=True):
    qT_d, cb_d, cf_d, xT_d, xo_d = T["qT"], T["cb"], T["cf"], T["xT"], T["xo"]
    w_o, w_in, w_out = T["w_o"], T["w_in"], T["w_out"]
    qi_d, wi_d, ao_d = T.get("qi"), T.get("wi"), T.get("ao")
    with ExitStack() as es:
        K = Ctx(nc, es, pfx)
        setup_common(K)
        PS = K.PS
        outT = K.sb("outT_sb", [128, KD, TL], BF16)
        vecs = K.sb("vecs_sb", [128, 112], F32)
        s_in = K.sem("s_in")
        s_vec = K.sem("s_vec")
        t_vl = T["vec_loader"](K, vecs, s_vec)
        t_v1 = K.emit("dve", lambda e: e.tensor_tensor(out=vecs[:, 0:16], in0=vecs[:, 0:16], in1=vecs[:, 64:80], op=ALU.mult), waits=[t_vl])
        t_v2 = K.emit("dve", lambda e: e.scalar_tensor_tensor(out=vecs[:, 16:32], in0=vecs[:, 16:32], scalar=1.0, in1=vecs[:, 80:96], op0=ALU.add, op1=ALU.mult), waits=[t_v1])
        t_v3 = K.emit("dve", lambda e: e.tensor_tensor(out=vecs[:, 48:64], in0=vecs[:, 48:64], in1=vecs[:, 96:112], op=ALU.mult), waits=[t_v2])
        for eng in ("act", "dve", "pool", "pe"):
            K.wait(eng, [t_v3])
        with ExitStack() as es2:
            K2 = K
            old_es = K.es
            K.es = es2
            cb = K.sb("cb_sb", [128, 3328], BF16)
            cf = K.sb("cf_sb", [128, 896], F32)
            K.dma("sp", cb[:, :], cb_d, s_in)
            K.dma("sp", cf[:, :], cf_d, s_in)
            cst = {}
            if kind == "B":
                qT = K.sb("qT_sb", [128, KD, TL], BF16)
                K.dma("sp", qT[:, :, :], qT_d.rearrange("(k p) t -> p k t", p=128), s_in)
                cst = {"ones_bf": cb[:, 128:256],
                       "pT_ring": Ring([K.sb(f"pT{i}", [128, 512], BF16) for i in range(4)]),
                       "rden_ring": Ring([K.sb(f"rden{i}", [128, 512], F32) for i in range(2)])}
                kbuf = [K.sb(f"kTb{i}", [128, 8, TL], BF16) for i in range(2)]
                vbuf = [K.sb(f"vb{i}", [128, 64, 128], BF16) for i in range(2)]
                ksem = [K.sem(f"s_kv{i}") for i in range(2)]
            in_tok = Tok(s_in, s_in.count)
            for eng in ("pe", "act", "dve", "pool"):
                K.wait(eng, [in_tok])
            ident_bf = cb[:, 0:128]
            ident_f = cf[:, 0:128]
            kvfree = [[], []]
            kv_loaded = {}

            def load_kv(g):
                j = g % 2
                t = T["kv_loader"](K, g, kbuf[j], vbuf[j], ksem[j], kvfree[j])
                kvfree[j] = []
                kv_loaded[g] = t

            if kind == "B":
                ksa = K.sb("ksa", [128, 8, 512], F32)
                kmean = K.sb("kmean", [128, 512], BF16)
                gm = K.sb("gm", [128, 256], F32)
                top8 = K.sb("top8", [128, 8, 8], F32)
                sel = K.sb("sel", [128, 256], F32)
                selbT = [K.sb(f"selbT{i}", [32, TL], BF16) for i in range(2)]
                selb_free = [[], []]
                s_ks = K.sem("s_ks")
                t_ks = T["ks_loader"](K, ksa, s_ks)
                t = t_ks
                for r in range(1, 8):
                    t = K.emit("dve", lambda e, r=r: e.tensor_tensor(out=ksa[:, 0, :], in0=ksa[:, 0, :], in1=ksa[:, r, :], op=ALU.add), waits=[t])
                t_km = K.emit("dve", lambda e: e.tensor_scalar(out=kmean[:, :], in0=ksa[:, 0, :], scalar1=1.0 / 256, scalar2=None, op0=ALU.mult), waits=[t])
                load_kv(0)
                gate_state = {"bank_free": [], "sel_tok": {}}

                def moba_select(h):
                    gps = PS[3]
                    j = h % 2
                    tg_ = None
                    for qt in range(8):
                        tg_ = K.emit("pe", lambda e, qt=qt: e.matmul(gps[:, qt * 32:(qt + 1) * 32], lhsT=qT[:, h, qt * 128:(qt + 1) * 128],
                                                                      rhs=kmean[:, h * 32:(h + 1) * 32], start=True, stop=True),
                                     waits=[t_km] + (gate_state["bank_free"] if qt == 0 else []), sig=(qt == 7))
                    t1 = K.emit("dve", lambda e: e.tensor_tensor(out=gm[:, :], in0=gps[:, 0:256], in1=cf[:, 640:896], op=ALU.add), waits=[tg_])
                    tt = t1
                    for qt in range(8):
                        tt = K.emit("dve", lambda e, qt=qt: e.max(out=top8[:, qt, :], in_=gm[:, qt * 32:(qt + 1) * 32]), waits=[tt])
                    for qt in range(8):
                        tt = K.emit("dve", lambda e, qt=qt: e.tensor_scalar(out=sel[:, qt * 32:(qt + 1) * 32], in0=gm[:, qt * 32:(qt + 1) * 32],
                                                                              scalar1=top8[:, qt, 2:3], scalar2=None, op0=ALU.is_ge), waits=[tt])
                    tt = K.emit("dve", lambda e: e.tensor_tensor(out=sel[:, :], in0=sel[:, :], in1=cf[:, 128:384], op=ALU.mult), waits=[tt])
                    tt = K.emit("dve", lambda e: e.tensor_tensor(out=sel[:, :], in0=sel[:, :], in1=cf[:, 384:640], op=ALU.add), waits=[tt])
                    tt = K.emit("dve", lambda e: e.tensor_scalar(out=sel[:, :], in0=sel[:, :], scalar1=-1.0, scalar2=-NEG, op0=ALU.add, op1=ALU.mult), waits=[tt])
                    tlast = None
                    for half in range(2):
                        tp = None
                        for q4 in range(4):
                            qt = half * 4 + q4
                            tp = K.emit("pe", lambda e, qt=qt, q4=q4: e.transpose(out=gps[0:32, q4 * 128:(q4 + 1) * 128], in_=sel[:, qt * 32:(qt + 1) * 32],
                                                                                identity=ident_f),
                                        waits=[tt] + ([t1] if half == 0 and q4 == 0 else []) + ([tlast] if half == 1 and q4 == 0 else []), sig=(q4 == 3))
                        tlast = K.emit("act", lambda e, half=half: e.activation(out=selbT[j][:, half * 512:(half + 1) * 512], in_=gps[0:32, 0:512], func=AF.Copy),
                                       waits=[tp] + selb_free[j])
                    selb_free[j] = []
                    gate_state["bank_free"] = [tlast]
                    gate_state["sel_tok"][h] = tlast

                def get_kv(h):
                    if h + 1 < 16:
                        load_kv(h + 1)
                    moba_select(h)
                    j = h % 2

                    def rel(hh, toks):
                        kvfree[hh % 2] = list(toks)
                        selb_free[hh % 2] = list(toks)
                    return kbuf[j], vbuf[j], kv_loaded[h], rel

                def qf_of(h):
                    return (lambda lo, hi: qT[:, h, lo:hi]), in_tok

                def bias_mm(h, tg, m, r, c0):
                    j = h % 2
                    stk = gate_state["sel_tok"][h]
                    lst = []
                    qt0 = 4 * tg
                    has_diag = (m >= qt0)
                    lst.append((lambda e, st, m=m, c0=c0, tg=tg, j=j, has_diag=has_diag: e.matmul(
                        st[:, c0:512], lhsT=cb[0:32, 256 + m * 128:256 + (m + 1) * 128], rhs=selbT[j][:, tg * 512 + c0:tg * 512 + 512],
                        start=False, stop=(not has_diag)), [stk]))
                    if has_diag:
                        lst.append((lambda e, st, r=r, c0=c0: e.matmul(st[:, c0:c0 + 128], lhsT=ident_bf, rhs=cb[:, 1280 + r * 128:1280 + (r + 1) * 128],
                                                                      start=False, stop=True), []))
                    return lst

                attention_core(K, list(range(16)), [0, 1], get_kv, qf_of, bias_mm, outT, cst, PS[0:3], [(PS[4], PS[5]), (PS[6], PS[7])])

            if kind == "A":
                wi_sb = K.sb("wi_sb", [128, 8, 16], F32)
                maskbT = K.sb("maskbT", [128, 208, 128], BF16)
                sm = K.sb("sm", [128, 8], F32)
                s_a = K.sem("s_a")
                K.dma("sp", wi_sb[:, :, :], wi_d.rearrange("(a p) c -> p a c", p=128), s_a)
                for tg in range(2):
                    qt0 = 4 * tg
                    offs = {}
                    o_ = 0
                    for qt in range(qt0, qt0 + 4):
                        offs[qt] = o_
                        o_ += 8 * (qt + 1)
                    with ExitStack() as es3:
                        K.es = es3
                        qiT = K.sb("qiT_sb_" + str(tg), [128, 8, 512], BF16)
                        kiT = K.sb("kiT_sb_" + str(tg), [128, 8, 8, 128], BF16)
                        isc = K.sb("isc_" + str(tg), [128, 8192], F32)
                        junk = K.sb("junk_" + str(tg), [128, 8192], BF16)
                        diag = K.sb("diag_" + str(tg), [128, 16, 128], BF16)
                        rl_ring = Ring([K.sb(f"rl{i}_{tg}", [128, 512], BF16) for i in range(4)])
                        K.dma("sp", qiT[:, :, :], qi_d[:, tg * 512:(tg + 1) * 512].rearrange("(k p) t -> p k t", p=128), s_a)
                        T["ki_loader"](K, kiT, s_a)
                        a_tok = Tok(s_a, s_a.count)
                        for eng in ("pe", "act", "dve"):
                            K.wait(eng, [a_tok])
                        d_ring = Ring(PS[0:3])
                        i_ring = Ring(PS[3:5])
                        t_ring = Ring(PS[5:7])
                        chain = None
                        for qt in range(qt0, qt0 + 4):
                            tl = (qt - qt0) * 128
                            ncol = 1024 * (qt + 1)
                            td = None
                            for hh in range(16):
                                td = K.emit("dve", lambda e, hh=hh, qt=qt: e.tensor_scalar(out=diag[:, hh, :], in0=ident_bf, scalar1=wi_sb[:, qt, hh:hh + 1],
                                                                                            scalar2=None, op0=ALU.mult), waits=[chain, cst.get("diag_free")])
                            ev_toks = []
                            iwork = []
                            for grp in range(2 * (qt + 1)):
                                for hh in range(16):
                                    iwork.append({"grp": grp, "hh": hh})
                            istate = {"ips": None, "ti": None}

                            def istage_a(w):
                                grp, hh = w["grp"], w["hh"]
                                m = grp // 2
                                r0 = (grp % 2) * 4
                                hp = hh // 2
                                po = (hh % 2) * 64
                                dj, dps, dfree = d_ring.next()
                                t_d = K.emit("pe", lambda e: e.matmul(dps[:, 0:512], lhsT=qiT[po:po + 64, hp, tl:tl + 128], rhs=kiT[po:po + 64, m, r0:r0 + 4, :],
                                                                      start=True, stop=True), waits=dfree)
                                rj, rl, rfree = rl_ring.next()
                                t_r = K.emit("act", lambda e: e.activation(out=rl[:, :], in_=dps[:, 0:512], func=AF.Relu), waits=[t_d] + rfree)
                                d_ring.release(dj, [t_r])
                                w["rj"], w["rl"], w["t_r"] = rj, rl, t_r

                            def istage_b(w):
                                grp, hh = w["grp"], w["hh"]
                                if hh == 0:
                                    istate["ips"] = i_ring.next()
                                ij, ips, ifree = istate["ips"]
                                rl = w["rl"]
                                ti = K.emit("pe", lambda e: e.matmul(ips[:, 0:512], lhsT=diag[:, hh, :], rhs=rl[:, :], start=(hh == 0), stop=(hh == 15)),
                                            waits=[w["t_r"], td] + (ifree if hh == 0 else []))
                                rl_ring.release(w["rj"], [ti])
                                istate["ti"] = ti
                                if hh == 15:
                                    t_ev = K.emit("act", lambda e: e.activation(out=isc[:, grp * 512:(grp + 1) * 512], in_=ips[:, 0:512], func=AF.Copy),
                                                  waits=[ti, chain])
                                    i_ring.release(ij, [t_ev])
                                    ev_toks.append(t_ev)

                            ILA = 2
                            for n_, w in enumerate(iwork):
                                istage_a(w)
                                if n_ >= ILA:
                                    istage_b(iwork[n_ - ILA])
                            for w in iwork[max(0, len(iwork) - ILA):]:
                                istage_b(w)
                            ti = istate["ti"]
                            cst["diag_free"] = ti
                            t = K.emit("dve", lambda e, ncol=ncol: e.tensor_reduce(out=sm[:, 1:2], in_=isc[:, 0:ncol], axis=AX.X, op=ALU.max), waits=ev_toks + [chain])
                            t = K.emit("dve", lambda e, ncol=ncol: e.tensor_reduce(out=sm[:, 0:1], in_=isc[:, 0:ncol], axis=AX.X, op=ALU.min), waits=[t])
                            for r in range(8):
                                c_ = qt * 1024 + r * 128
                                t = K.emit("dve", lambda e, c_=c_, r=r: e.tensor_tensor(out=isc[:, c_:c_ + 128], in0=isc[:, c_:c_ + 128],
                                                                                         in1=cb[:, 2304 + r * 128:2304 + (r + 1) * 128], op=ALU.add), waits=[t])
                            for it in range(22):
                                t = K.emit("dve", lambda e: e.tensor_scalar(out=sm[:, 2:3], in0=sm[:, 0:1], scalar1=sm[:, 1:2], scalar2=0.5, op0=ALU.add, op1=ALU.mult), waits=[t])
                                t = K.emit("dve", lambda e, ncol=ncol: e.tensor_scalar(out=junk[:, 0:ncol], in0=isc[:, 0:ncol], scalar1=sm[:, 2:3], scalar2=None,
                                                                                        op0=ALU.is_ge, op1=ALU.add, accum_out=sm[:, 3:4]), waits=[t])
                                t = K.emit("dve", lambda e: e.tensor_scalar(out=sm[:, 4:5], in0=sm[:, 3:4], scalar1=255.5, scalar2=None, op0=ALU.is_ge), waits=[t])
                                t = K.emit("dve", lambda e: e.tensor_tensor(out=sm[:, 5:6], in0=sm[:, 2:3], in1=sm[:, 0:1], op=ALU.subtract), waits=[t])
                                t = K.emit("dve", lambda e: e.tensor_tensor(out=sm[:, 6:7], in0=sm[:, 4:5], in1=sm[:, 5:6], op=ALU.mult), waits=[t])
                                t = K.emit("dve", lambda e: e.tensor_tensor(out=sm[:, 0:1], in0=sm[:, 0:1], in1=sm[:, 6:7], op=ALU.add), waits=[t])
                                t = K.emit("dve", lambda e: e.tensor_tensor(out=sm[:, 1:2], in0=sm[:, 2:3], in1=sm[:, 6:7], op=ALU.add), waits=[t])
                            t = K.emit("dve", lambda e, ncol=ncol: e.tensor_scalar(out=isc[:, 0:ncol], in0=isc[:, 0:ncol], scalar1=sm[:, 0:1], scalar2=-1.0,
                                                                                    op0=ALU.is_ge, op1=ALU.add), waits=[t])
                            ntile = 8 * (qt + 1)
                            t_last = None
                            for b4 in range(ntile // 4):
                                tj, tps, tfree = t_ring.next()
                                tp = None
                                for q4 in range(4):
                                    ti_ = b4 * 4 + q4
                                    tp = K.emit("pe", lambda e, tps=tps, q4=q4, ti_=ti_: e.transpose(out=tps[:, q4 * 128:(q4 + 1) * 128], in_=isc[:, ti_ * 128:(ti_ + 1) * 128],
                                                                                                    identity=ident_f), waits=[t] + (tfree if q4 == 0 else []), sig=(q4 == 3))
                                base = offs[qt] + b4 * 4
                                t_last = K.emit("act", lambda e, tps=tps, base=base: e.activation(
                                    out=maskbT[:, base:base + 4, :], in_=tps[:, 0:512].rearrange("p (a b) -> p a b", b=128), func=AF.Copy, scale=-NEG),
                                    waits=[tp, cst.get("mask_free")])
                                t_ring.release(tj, [t_last])
                            chain = K.emit("dve", lambda e: e.memset(sm[:, 7:8], 0.0), waits=[t_last, t])
                        K.flush()
                        K.es = es2
                    with ExitStack() as es4:
                        K.es = es4
                        pcst = {"ones_bf": cb[:, 128:256],
                                "pT_ring": Ring([K.sb(f"pT{i}_{tg}", [128, 512], BF16) for i in range(4)]),
                                "rden_ring": Ring([K.sb(f"rden{i}_{tg}", [128, 512], F32) for i in range(2)])}
                        qT = K.sb("qT_sb_" + str(tg), [128, KD, TL], BF16)
                        s_q = K.sem(f"s_q{tg}")
                        q_tok = K.dma("sp", qT[:, :, :], qT_d.rearrange("(k p) t -> p k t", p=128), s_q)
                        kbuf = [K.sb(f"kTb{i}_{tg}", [128, 8, TL], BF16) for i in range(2)]
                        vbuf = [K.sb(f"vb{i}_{tg}", [128, 64, 128], BF16) for i in range(2)]
                        ksem = [K.sem(f"s_kv{i}_{tg}") for i in range(2)]
                        kvfree = [[], []]
                        kv_loaded = {}

                        def load_kv(g, kbuf=kbuf, vbuf=vbuf, ksem=ksem, kvfree=kvfree, kv_loaded=kv_loaded):
                            j = g % 2
                            kv_loaded[g] = T["kv_loader"](K, g, kbuf[j], vbuf[j], ksem[j], kvfree[j])
                            kvfree[j] = []

                        load_kv(0)

                        def get_kv(h, kbuf=kbuf, vbuf=vbuf, kvfree=kvfree, kv_loaded=kv_loaded, load_kv=load_kv):
                            g = h // 4
                            if h % 4 == 0 and g + 1 < 4:
                                load_kv(g + 1)

                            def rel(hh, toks):
                                if hh % 4 == 3:
                                    kvfree[(hh // 4) % 2] = list(toks)
                            return kbuf[g % 2], vbuf[g % 2], kv_loaded[g], rel

                        def qf_of(h, qT=qT, q_tok=q_tok):
                            return (lambda lo, hi: qT[:, h, lo:hi]), q_tok

                        def bias_mm(h, tg_, m, r, c0, offs=offs, qt0=qt0):
                            lst = []
                            for qt in range(max(m, qt0), qt0 + 4):
                                cc = (qt - qt0) * 128
                                lst.append((lambda e, st, cc=cc, qt=qt, m=m, r=r: e.matmul(st[:, cc:cc + 128], lhsT=ident_bf, rhs=maskbT[:, offs[qt] + m * 8 + r, :],
                                                                                         start=False, stop=(qt == qt0 + 3)), []))
                            return lst

                        attention_core(K, list(range(16)), [tg], get_kv, qf_of, bias_mm, outT, pcst, PS[0:3], [(PS[3], PS[4]), (PS[5], PS[6])])
                        cst["out_tok"] = pcst["out_tok"]
                        cst["mask_free"] = pcst["out_tok"]
                        K.flush()
                        K.es = es2
            K.flush()
            K.es = old_es
        st = None
        if with_post:
            K.wait("pe", [in_tok])
            st = make_post_state(K)
            st["outT_tok"] = cst["out_tok"]
            st["ws"].plan(post_plan(w_o, w_in, w_out) * 2)
            last = None
            for half in range(2):
                last = post_sublayers(K, half, outT, xT_d, xo_d, w_o, w_in, w_out, st["ws"],
                                      vecs[:, 0:16], vecs[:, 16:32], vecs[:, 32:48], vecs[:, 48:64], PS, st)
            K.wait("sp", [last])
            K.flush()
        else:
            s_o = K.sem("s_o")
            t_o = K.dma("sp", ao_d.rearrange("(k p) t -> p k t", p=128), outT[:, :, :], s_o, waits=[cst["out_tok"]])
            K.wait("sp", [t_o])
            K.flush()


def _opt(ap):
    return ap.opt() if hasattr(ap, "opt") else ap


def emit_ag(nc, pairs, pfx):
    with ExitStack() as es:
        K = Ctx(nc, es, pfx)
        s = K.sem("cc")
        t = None
        for (a, b) in pairs:
            t = K.collective("AllGather", _opt(a), _opt(b), s, waits=[t])
        K.wait("pool", [t])
        K.flush()


def build_fused(nl=4, l0=0):
    nc = bass.Bass("TRN2", target_bir_lowering=False)

    def ext(name, shape, dt):
        return nc.dram_tensor(name, shape, dt, kind="ExternalInput").ap()

    _int = {}

    def internal(name, shape, dt):
        if name[-2] == "_" and name[-1] in "0123":
            name = name[:-1] + str(int(name[-1]) % 2)
        if name not in _int:
            _int[name] = nc.dram_tensor(name, shape, dt).ap()
        return _int[name]

    E = {"xT": ext("xT", [D, TL], F32), "posrep": ext("posrep", [128, TL], I32), "cT": ext("cT", [128, KD], F32),
         "adaw": ext("adaw", [D, 6144], F32), "adab": ext("adab", [128, 48], F32), "normg": ext("normg", [128, 256], F32),
         "fvec": ext("fvec", [128, 2], F32), "rconst": ext("rconst", [128, 256], BF16), "cb": ext("cb", [128, 3328], BF16),
         "cf": ext("cf", [128, 896], F32), "a_w_in": ext("a_w_in", [2, D, 4176], F32), "a_w_o": ext("a_w_o", [2, D, D], F32),
         "b_w_in": ext("b_w_in", [2, D, 6144], F32), "b_w_o": ext("b_w_o", [2, D, D], F32),
         "ffn_w_in": ext("ffn_w_in", [4, D, 2 * DFF], F32), "ffn_w_out": ext("ffn_w_out", [4, DFF, D], F32)}
    xo = nc.dram_tensor("xo", [D, TL], F32, kind="ExternalOutput").ap()
    modo = internal("modo", [128, 48], F32)
    modall = internal("modall", [1024, 48], F32)
    xbuf = [internal("xbufa", [D, TL], F32), internal("xbufb", [D, TL], F32)]
    emit_mod(nc, {"cT": E["cT"], "adaw": E["adaw"], "adab": E["adab"], "modo": modo}, "m_")
    emit_ag(nc, [(modo, modall)], "mg_")
    for i in range(l0, nl):
        kind = "A" if i % 2 == 0 else "B"
        nkv = 4 if kind == "A" else 16
        x_in = E["xT"] if i == l0 else xbuf[(i - 1) % 2]
        x_out = xo if i == nl - 1 else xbuf[i % 2]
        q_i = internal(f"q_{i}", [D, TL], BF16)
        k_i = internal(f"k_{i}", [nkv * 128, TL], BF16)
        kall = internal(f"kall_{i}", [8 * nkv * 128, TL], BF16)
        v_i = internal(f"v_{i}", [TL, nkv * 128], BF16)
        vall = internal(f"vall_{i}", [8 * TL, nkv * 128], BF16)
        r0 = 2 * i * 128
        r1 = (2 * i + 1) * 128

        def pre_vec_loader(K, vecs, sem, i=i, r0=r0):
            K.dma("sp", vecs[:, 0:16], modall[r0:r0 + 128, 16:32], sem)
            K.dma("sp", vecs[:, 16:32], modall[r0:r0 + 128, 0:16], sem)
            K.dma("sp", vecs[:, 32:48], E["normg"][:, (i * 4) * 16:(i * 4 + 1) * 16], sem)
            return K.dma("sp", vecs[:, 48:50], E["fvec"], sem)

        def att_vec_loader(K, vecs, sem, i=i, r0=r0, r1=r1):
            K.dma("sp", vecs[:, 0:16], modall[r0:r0 + 128, 32:48], sem)
            K.dma("sp", vecs[:, 16:32], modall[r1:r1 + 128, 16:32], sem)
            K.dma("sp", vecs[:, 32:48], modall[r1:r1 + 128, 0:16], sem)
            K.dma("sp", vecs[:, 48:64], modall[r1:r1 + 128, 32:48], sem)
            return K.dma("sp", vecs[:, 64:112], E["normg"][:, (i * 4 + 1) * 16:(i * 4 + 4) * 16], sem)

        Tp = {"xT": x_in, "vec_loader": pre_vec_loader, "posrep": E["posrep"], "rconst": E["rconst"],
              "w": (E["a_w_in"] if kind == "A" else E["b_w_in"])[i // 2], "q": q_i, "k": k_i, "v": v_i}
        pairs = [(k_i, kall), (v_i, vall)]
        kall_v = kall.rearrange("(r g d) t -> g d r t", r=8, g=nkv)
        vall_v = vall.rearrange("(r m p) (g d) -> g p r m d", r=8, m=8, g=nkv)

        def kv_loader(K, g, kdst, vdst, sem, waits, kall_v=kall_v, vall_v=vall_v):
            t = K.dma("sp", kdst[:, :, :], kall_v[g], sem, waits=waits)
            for r in range(8):
                t = K.dma("sp", vdst[:, r * 8:(r + 1) * 8, :], vall_v[g][:, r], sem, waits=waits)
            return t

        Ta = {"qT": q_i, "kv_loader": kv_loader, "cb": E["cb"], "cf": E["cf"], "vec_loader": att_vec_loader, "xT": x_in, "xo": x_out,
              "w_o": (E["a_w_o"] if kind == "A" else E["b_w_o"])[i // 2], "w_in": E["ffn_w_in"][i], "w_out": E["ffn_w_out"][i]}
        if kind == "A":
            qi_i = internal(f"qi_{i}", [1024, TL], BF16)
            ki_i = internal(f"ki_{i}", [64, TL], BF16)
            kiall = internal(f"kiall_{i}", [512, TL], BF16)
            wi_i = internal(f"wi_{i}", [TL, 16], F32)
            Tp.update({"qi": qi_i, "ki": ki_i, "wi": wi_i})
            pairs.append((ki_i, kiall))
            kiall_v = kiall.rearrange("(r d) (m p) -> d m r p", r=8, m=8)

            def ki_loader(K, kiT, sem, kiall_v=kiall_v):
                K.dma("sp", kiT[0:64, :, :, :], kiall_v, sem)
                K.dma("sp", kiT[64:128, :, :, :], kiall_v, sem)
            Ta.update({"qi": qi_i, "wi": wi_i, "ki_loader": ki_loader})
        else:
            ks_i = internal(f"ks_{i}", [128, 512], F32)
            ksall = internal(f"ksall_{i}", [1024, 512], F32)
            Tp["ksum"] = ks_i
            pairs.append((ks_i, ksall))
            ksall_v = ksall.rearrange("(r p) c -> p r c", p=128)
            Ta["ks_loader"] = lambda K, ksa, sem, ksall_v=ksall_v: K.dma("sp", ksa[:, :, :], ksall_v, sem)
        emit_pre(nc, kind, Tp, f"p{i}_")
        emit_ag(nc, pairs, f"g{i}_")
        emit_att(nc, kind, Ta, f"a{i}_")
    return nc


_PROGS = {}


def _prog(name, fn):
    if name not in _PROGS:
        _PROGS[name] = fn()
    return _PROGS[name]


def _run(nc, in_maps):
    res = run_bass_kernel_spmd(nc, in_maps, core_ids=list(range(NCORES)))
    return res.results


def kernel(x, c, positions, a_w_in, a_w_o, b_w_in, b_w_o, ada_w, ada_b, norm_g, ffn_w_in, ffn_w_out):
    x = np.asarray(x, np.float32)
    c = np.asarray(c, np.float32)
    positions = np.asarray(positions, np.int32)
    a_w_in, a_w_o, b_w_in, b_w_o = [np.asarray(a, np.float32) for a in (a_w_in, a_w_o, b_w_in, b_w_o)]
    ada_w, ada_b, norm_g = [np.asarray(a, np.float32) for a in (ada_w, ada_b, norm_g)]
    ffn_w_in, ffn_w_out = np.asarray(ffn_w_in, np.float32), np.asarray(ffn_w_out, np.float32)
    C = NCORES
    cT = vec_layout(c[0])
    maps = []
    for r in range(C):
        i, hf = r // 2, r % 2
        maps.append({"cT": cT, "adaw": np.ascontiguousarray(ada_w[i][:, hf * 6144:(hf + 1) * 6144]),
                     "adab": np.ascontiguousarray(ada_b[i][hf * 6144:(hf + 1) * 6144].reshape(48, 128).T)})
    mo = _run(_prog("mod", build_mod), maps)
    mod = np.zeros((4, 6, 128, 16), np.float32)
    for i in range(4):
        for ch in range(96):
            mod[i, ch // 16, :, ch % 16] = np.asarray(mo[2 * i + ch // 48]["modo"])[:, ch % 48]
    fvec, R = rope_consts()
    xT = [np.ascontiguousarray(x[0, r::C, :].T) for r in range(C)]
    posrep = [np.ascontiguousarray(np.broadcast_to(positions[0, r::C][None, :], (128, TL))) for r in range(C)]
    consts = [att_consts(r) for r in range(C)]
    for i in range(4):
        kind = "A" if i % 2 == 0 else "B"
        sh1, sc1, g1, sh2, sc2, g2 = [mod[i, j] for j in range(6)]
        ng = [vec_layout(norm_g[i, j]) for j in range(4)]
        if kind == "A":
            w = a_w_in[i // 2]
            cuts = np.cumsum([2048, 512, 512, 1024, 64])
            wq, wk, wv, wqi, wki, wwi = np.split(w, cuts, axis=1)
            wperm = np.ascontiguousarray(np.concatenate([wq, wk, wqi, wki, wv, wwi], axis=1))
            w_o = a_w_o[i // 2]
            nkv = 4
        else:
            wperm = np.ascontiguousarray(b_w_in[i // 2])
            w_o = b_w_o[i // 2]
            nkv = 16
        vecs_pre = np.ascontiguousarray(np.concatenate([sc1, sh1, ng[0], fvec], axis=1).astype(np.float32))
        maps = [{"xT": xT[r], "vecs": vecs_pre, "posrep": posrep[r], "w": wperm, "rconst": R} for r in range(C)]
        pr = _run(_prog("pre" + kind, lambda: build_pre(kind)), maps)
        kT = np.stack([np.asarray(pr[r]["kT"]) for r in range(C)])
        kTall = np.ascontiguousarray(kT.reshape(C, nkv, 128, TL).transpose(1, 2, 0, 3))
        vv = np.stack([np.asarray(pr[r]["v"]) for r in range(C)])
        vall = np.ascontiguousarray(vv.reshape(C, 8, 128, nkv, 128).transpose(3, 2, 0, 1, 4).reshape(nkv, 128, 64, 128))
        vecs_att = np.ascontiguousarray(np.concatenate([g1, sc2, sh2, g2, ng[1], ng[2], ng[3]], axis=1).astype(np.float32))
        maps = []
        for r in range(C):
            m_ = {"qT": np.asarray(pr[r]["qT"]), "kTall": kTall, "vall": vall, "cb": consts[r][0], "cf": consts[r][1], "vecs": vecs_att,
                  "xT": xT[r], "w_o": w_o, "w_in": ffn_w_in[i], "w_out": ffn_w_out[i]}
            maps.append(m_)
        if kind == "A":
            kiT = np.stack([np.asarray(pr[r]["kiT"]) for r in range(C)])
            kiall = np.ascontiguousarray(kiT.reshape(C, 64, 8, 128).transpose(1, 2, 0, 3))
            for r in range(C):
                maps[r]["qiT"] = np.asarray(pr[r]["qiT"])
                maps[r]["kiall"] = kiall
                maps[r]["wi"] = np.asarray(pr[r]["wi"])
        else:
            ks = np.stack([np.asarray(pr[r]["ksum"]) for r in range(C)])
            ksall = np.ascontiguousarray(ks.transpose(1, 0, 2))
            for r in range(C):
                maps[r]["ksall"] = ksall
        ar = _run(_prog("att" + kind, lambda: build_att(kind)), maps)
        xT = [np.asarray(ar[r]["xo"]) for r in range(C)]
    out = np.zeros((1, S, D), np.float32)
    for r in range(C):
        out[0, r::C, :] = xT[r].T
    return out


def kernel_fused(x, c, positions, a_w_in, a_w_o, b_w_in, b_w_o, ada_w, ada_b, norm_g, ffn_w_in, ffn_w_out):
    x = np.asarray(x, np.float32)
    c = np.asarray(c, np.float32)
    positions = np.asarray(positions, np.int32)
    a_w_in, a_w_o, b_w_in, b_w_o = [np.asarray(a, np.float32) for a in (a_w_in, a_w_o, b_w_in, b_w_o)]
    ada_w, ada_b, norm_g = [np.asarray(a, np.float32) for a in (ada_w, ada_b, norm_g)]
    ffn_w_in, ffn_w_out = np.ascontiguousarray(ffn_w_in, np.float32), np.ascontiguousarray(ffn_w_out, np.float32)
    C = NCORES
    cuts = np.cumsum([2048, 512, 512, 1024, 64])
    perm = []
    for l in range(2):
        wq, wk, wv, wqi, wki, wwi = np.split(a_w_in[l], cuts, axis=1)
        perm.append(np.concatenate([wq, wk, wqi, wki, wv, wwi], axis=1))
    a_w_in_p = np.ascontiguousarray(np.stack(perm))
    fvec, R = rope_consts()
    normg = np.ascontiguousarray(np.concatenate([vec_layout(norm_g[i, j]) for i in range(4) for j in range(4)], axis=1))
    cT = vec_layout(c[0])
    a_w_o = np.ascontiguousarray(a_w_o)
    b_w_in = np.ascontiguousarray(b_w_in)
    b_w_o = np.ascontiguousarray(b_w_o)
    maps = []
    for r in range(C):
        i, hf = r // 2, r % 2
        cb, cf = att_consts(r)
        maps.append({"xT": np.ascontiguousarray(x[0, r::C, :].T),
                     "posrep": np.ascontiguousarray(np.broadcast_to(positions[0, r::C][None, :], (128, TL))),
                     "cT": cT, "adaw": np.ascontiguousarray(ada_w[i][:, hf * 6144:(hf + 1) * 6144]),
                     "adab": np.ascontiguousarray(ada_b[i][hf * 6144:(hf + 1) * 6144].reshape(48, 128).T),
                     "normg": normg, "fvec": fvec, "rconst": R, "cb": cb, "cf": cf,
                     "a_w_in": a_w_in_p, "a_w_o": a_w_o, "b_w_in": b_w_in, "b_w_o": b_w_o,
                     "ffn_w_in": ffn_w_in, "ffn_w_out": ffn_w_out})
    res = _run(_prog("fused", lambda: build_fused(4, 0)), maps)
    out = np.zeros((1, S, D), np.float32)
    for r in range(C):
        out[0, r::C, :] = np.asarray(res[r]["xo"]).T
    return out
```
